# Optimizing a Trainium2 kernel written in Bass

```python
import jax, jax.numpy as jnp
from jax import lax
import numpy as np

D_MODEL = 1024
BATCH = 8
SEQ = 4096
DEPTH = 4

MIX_WIDTH = D_MODEL
GLA_WIDTH = MIX_WIDTH // 2
CONV_WIDTH = MIX_WIDTH - GLA_WIDTH
GLA_HEADS = 4
GLA_DV = GLA_WIDTH // GLA_HEADS
GLA_DK = GLA_DV // 2
GLA_KEY_WIDTH = GLA_HEADS * GLA_DK
GATE_RANK = 16
GATE_TAU = 16.0
CHUNK = 64
CONV_GROUPS = 8
CONV_K = 3
D_FF = 2816
EPS = 1e-6

SPLIT_SIZES = (GLA_KEY_WIDTH,
               GLA_KEY_WIDTH,
               GLA_WIDTH,
               GLA_WIDTH,
               GATE_RANK,
               CONV_WIDTH,
               CONV_WIDTH,
               CONV_WIDTH)
SPLIT_POINTS = tuple(int(v) for v in np.cumsum(SPLIT_SIZES)[:-1])
PROJ_WIDTH = int(sum(SPLIT_SIZES))

kernel_name = "hymba_style_gla_shortconv_macaron"


def rmsnorm(x, g):
    xf = x.astype(jnp.float32)
    y = xf * lax.rsqrt(jnp.mean(xf * xf, axis=-1, keepdims=True) + EPS)
    return (y * g.astype(jnp.float32)).astype(x.dtype)


def swiglu(x, w_gate, w_up, w_down):
    return (jax.nn.silu(x @ w_gate) * (x @ w_up)) @ w_down


def gla_chunked(q, k, v, logg):
    b_, t_, h_, dk = q.shape
    dv = v.shape[-1]
    n = t_ // CHUNK

    def to_chunks(a):
        return a.reshape(b_, n, CHUNK, h_, a.shape[-1]).transpose(1, 0, 3, 2, 4)

    qc, kc, vc, gc = to_chunks(q), to_chunks(k), to_chunks(v), to_chunks(logg)
    causal = jnp.tril(jnp.ones((CHUNK, CHUNK), dtype=bool))[None, None, :, :, None]

    def step(state, inp):
        qi, ki, vi, gi = inp
        bcum = jnp.cumsum(gi, axis=2)
        inter = jnp.einsum('bhck,bhkv->bhcv', qi * jnp.exp(bcum), state)
        diff = bcum[:, :, :, None, :] - bcum[:, :, None, :, :]
        decay = jnp.exp(jnp.where(causal, diff, -jnp.inf))
        scores = jnp.einsum('bhik,bhjk,bhijk->bhij', qi, ki, decay)
        out = inter + jnp.einsum('bhij,bhjv->bhiv', scores, vi)
        b_last = bcum[:, :, -1:, :]
        state = (state * jnp.exp(b_last[:, :, 0, :])[..., None]
                 + jnp.einsum('bhck,bhcv->bhkv', ki * jnp.exp(b_last - bcum), vi))
        return state, out

    s0 = jnp.zeros((b_, h_, dk, dv), jnp.float32)
    _, outs = lax.scan(step, s0, (qc, kc, vc, gc))
    return outs.transpose(1, 0, 3, 2, 4).reshape(b_, t_, h_, dv)


def causal_dwconv(u, w):
    c = u.shape[-1]
    return lax.conv_general_dilated(
        u, w.astype(u.dtype)[:, None, :], window_strides=(1,),
        padding=[(CONV_K - 1, 0)],
        dimension_numbers=('NWC', 'WIO', 'NWC'),
        feature_group_count=c)


def hybrid_mixer(h, w_in, w_a2, b_a, gla_out_norm, conv_w, w_out):
    b_, t_, _ = h.shape
    proj = h @ w_in
    q, k, v, g, a_lr, u, gate_b, gate_c = jnp.split(proj, SPLIT_POINTS, axis=-1)

    logg = jax.nn.log_sigmoid((a_lr @ w_a2 + b_a).astype(jnp.float32)) / GATE_TAU
    heads = lambda a, d: a.astype(jnp.float32).reshape(b_, t_, GLA_HEADS, d)
    o = gla_chunked(heads(q, GLA_DK) * (GLA_DK ** -0.5), heads(k, GLA_DK),
                    heads(v, GLA_DV), logg.reshape(b_, t_, GLA_HEADS, GLA_DK))
    o = o * lax.rsqrt(jnp.mean(o * o, axis=-1, keepdims=True) + EPS)
    o = (o * gla_out_norm.astype(jnp.float32)).reshape(b_, t_, GLA_WIDTH)
    o = o.astype(h.dtype) * jax.nn.silu(g)

    z = gate_b * causal_dwconv(gate_c * u, conv_w)

    return jnp.concatenate([o, z], axis=-1) @ w_out


def setup_inputs(seed: int = 0) -> dict:
    key = jax.random.key(seed)
    ks = jax.random.split(key, 20)
    f32 = jnp.float32

    def nrm(k, shape, fan_in):
        return jax.random.normal(k, shape, f32) * (fan_in ** -0.5)

    def gain(k, shape):
        return 1.0 + 0.02 * jax.random.normal(k, shape, f32)

    return {
        "x": jax.random.normal(ks[0], (BATCH, SEQ, D_MODEL), f32),
        "ffn1_norm": gain(ks[1], (DEPTH, D_MODEL)),
        "ffn1_w_gate": nrm(ks[2], (DEPTH, D_MODEL, D_FF), D_MODEL),
        "ffn1_w_up": nrm(ks[3], (DEPTH, D_MODEL, D_FF), D_MODEL),
        "ffn1_w_down": nrm(ks[4], (DEPTH, D_FF, D_MODEL), D_FF),
        "mix_norm": gain(ks[5], (DEPTH, D_MODEL)),
        "w_in": nrm(ks[6], (DEPTH, D_MODEL, PROJ_WIDTH), D_MODEL),
        "w_a2": nrm(ks[7], (DEPTH, GATE_RANK, GLA_KEY_WIDTH), GATE_RANK),
        "b_a": 0.01 * jax.random.normal(ks[8], (DEPTH, GLA_KEY_WIDTH), f32),
        "gla_out_norm": gain(ks[9], (DEPTH, GLA_DV)),
        "conv_w": nrm(ks[10], (DEPTH, CONV_K, CONV_WIDTH), CONV_K),
        "w_out": nrm(ks[11], (DEPTH, MIX_WIDTH, D_MODEL), MIX_WIDTH),
        "ffn2_norm": gain(ks[12], (DEPTH, D_MODEL)),
        "ffn2_w_gate": nrm(ks[13], (DEPTH, D_MODEL, D_FF), D_MODEL),
        "ffn2_w_up": nrm(ks[14], (DEPTH, D_MODEL, D_FF), D_MODEL),
        "ffn2_w_down": nrm(ks[15], (DEPTH, D_FF, D_MODEL), D_FF),
        "final_norm": gain(ks[16], (D_MODEL,)),
    }


def reference(x, ffn1_norm, ffn1_w_gate, ffn1_w_up, ffn1_w_down, mix_norm,
              w_in, w_a2, b_a, gla_out_norm, conv_w, w_out, ffn2_norm,
              ffn2_w_gate, ffn2_w_up, ffn2_w_down, final_norm):
    for l in range(DEPTH):
        x = x + 0.5 * swiglu(rmsnorm(x, ffn1_norm[l]),
                             ffn1_w_gate[l], ffn1_w_up[l], ffn1_w_down[l])
        x = x + hybrid_mixer(rmsnorm(x, mix_norm[l]), w_in[l], w_a2[l], b_a[l],
                             gla_out_norm[l], conv_w[l], w_out[l])
        x = x + 0.5 * swiglu(rmsnorm(x, ffn2_norm[l]),
                             ffn2_w_gate[l], ffn2_w_up[l], ffn2_w_down[l])
    return rmsnorm(x, final_norm)
```

```python
import numpy as np
from contextlib import ExitStack
import concourse.bass as bass
import concourse.mybir as mybir
from concourse.bass_utils import run_bass_kernel_spmd

F32 = mybir.dt.float32
BF16 = mybir.dt.bfloat16
AF = mybir.ActivationFunctionType
ALU = mybir.AluOpType

P = 128
D = 1024
DC = D // P
F = 2816
FS = 256
NFS = F // FS
HALF_SLABS = (6, 5)
PW = 3088
TT = 1024
SUB = 512
NS = TT // SUB
NCH = TT // P
EPS = 1e-6
RING = 3
COMPUTE = ("pe", "act", "dve", "pool")
ARENA_NAMES = {"act", "tmpf", "stage", "ostage", "yT", "wa2f", "qt", "kt", "vtok", "khat", "sg", "mixT",
               "Lb", "eb", "enb", "ebd", "AT", "sqo", "rso", "cu", "cacc", "Sbf"}


def is_arena(key):
    return (key[0] if isinstance(key, tuple) else key) in ARENA_NAMES


class Tracker:
    def __init__(self):
        self.streams = {k: [] for k in ("pe", "act", "dve", "pool", "sp")}
        self.nops = {k: 0 for k in COMPUTE}
        self.dma_cnt = {}
        self.lastw = {}
        self.readers = {}
        self.signal = {k: set() for k in COMPUTE}
        self.floor = {}

    def arena_switch(self):
        for table in (self.lastw, self.readers):
            for key in [k for k in table if is_arena(k)]:
                v = table.pop(key)
                items = [v] if table is self.lastw else list(v.items())
                for s, i in items:
                    if self.floor.get(s, -1) < i:
                        self.floor[s] = i

    def _deps(self, me, R, W):
        deps = {}

        def add(src, idx):
            if (src, idx) == me:
                return
            if src == "pe" and me[0] == "pe":
                return
            if deps.get(src, -1) < idx:
                deps[src] = idx

        if self.floor and any(is_arena(r) for r in list(R) + list(W)):
            for s, i in self.floor.items():
                add(s, i)
        for r in R:
            w = self.lastw.get(r)
            if w is not None:
                add(*w)
        for r in W:
            w = self.lastw.get(r)
            if w is not None:
                add(*w)
            rd = self.readers.get(r)
            if rd:
                for s, i in rd.items():
                    add(s, i)
        for r in R:
            rd = self.readers.setdefault(r, {})
            if rd.get(me[0], -1) < me[1]:
                rd[me[0]] = me[1]
        for r in W:
            self.lastw[r] = me
            self.readers[r] = {}
        for s, i in deps.items():
            if s in self.signal:
                self.signal[s].add(i)
        return deps

    def op(self, eng, fn, R=(), W=()):
        idx = self.nops[eng]
        self.nops[eng] += 1
        deps = self._deps((eng, idx), R, W)
        self.streams[eng].append(("op", idx, fn, deps))

    def dma(self, queue, key, n, fn, R=(), W=()):
        cnt = self.dma_cnt.get(key, 0) + n
        self.dma_cnt[key] = cnt
        deps = self._deps(("dma:" + key, cnt), R, W)
        self.streams[queue].append(("dma", key, fn, deps))

    def final_wait(self, stream):
        deps = {"dma:" + k: c for k, c in self.dma_cnt.items() if k.startswith("out")}
        self.streams[stream].append(("wait", None, None, deps))

    def emit(self, nc, es):
        sems = {}
        for e in COMPUTE:
            sems[e] = es.enter_context(nc.semaphore("s_" + e))
        for k in self.dma_cnt:
            sems["dma:" + k] = es.enter_context(nc.semaphore("d_" + k))
        rank = {}
        for e in COMPUTE:
            rank[e] = {idx: r + 1 for r, idx in enumerate(sorted(self.signal[e]))}

        def run(stream, eng):
            waited = {}
            for kind, a, fn, deps in self.streams[stream]:
                for s, i in deps.items():
                    val = rank[s][i] if s in rank else 16 * i
                    if waited.get(s, 0) >= val:
                        continue
                    waited[s] = val
                    eng.wait_ge(sems[s], val)
                if kind == "op":
                    ins = fn(eng)
                    if a in rank[stream]:
                        ins.then_inc(sems[stream], 1)
                elif kind == "dma":
                    fn(eng, sems["dma:" + a])

        block = es.enter_context(nc.Block())

        @block.tensor
        def _(e):
            run("pe", e)

        @block.scalar
        def _(e):
            run("act", e)

        @block.vector
        def _(e):
            run("dve", e)

        @block.gpsimd
        def _(e):
            run("pool", e)

        @block.sync
        def _(e):
            run("sp", e)


class Builder:
    def __init__(self, T, depth, stages=("f0", "mx", "f1")):
        self.stages = stages
        self.T = T
        self.depth = depth
        self.ntiles = T // TT
        self.nc = bass.Bass("TRN2", target_bir_lowering=False)
        self.tr = Tracker()
        self.bank_i = 0
        self.held = set()
        self.cur_phase = "io"
        self.slab_i = 0
        self.uid = 0
        self.cast_i = 0
        self.tick_i = 0
        self.phase_i = 0
        self.cur_pieces = []
        self.deferred = []

    def dram_in(self, name, shape):
        return self.nc.dram_tensor(name, list(shape), F32, kind="ExternalInput").ap()

    def sb(self, name, shape, dt):
        return self.es.enter_context(self.nc.sbuf_tensor(name, list(shape), dt))

    def bank(self, hold=False):
        i = self.bank_i
        while i in self.held:
            i = (i + 1) % 8
        self.bank_i = (i + 1) % 8
        if hold:
            self.held.add(i)
        return ("ps", i), self.ps[i]

    def phase(self, name):
        if self.cur_phase != name:
            self.tr.arena_switch()
            self.cur_phase = name

    def PE(self, fn, R=(), W=()):
        self.tr.op("pe", fn, R, W)

    def ACT(self, fn, R=(), W=()):
        self.tr.op("act", fn, R, W)

    def DVE(self, fn, R=(), W=()):
        self.tr.op("dve", fn, R, W)

    def POOL(self, fn, R=(), W=()):
        self.tr.op("pool", fn, R, W)

    def mm(self, out, lhsT, rhs, start, stop, R, W):
        self.PE(lambda e: e.matmul(out, lhsT, rhs, start=start, stop=stop), R, W)

    def build(self):
        nc = self.nc
        depth = self.depth
        T = self.T
        with ExitStack() as es:
            self.es = es
            es.enter_context(nc.allow_non_contiguous_dma(reason="small param loads"))
            self.x = self.dram_in("x", [T, D])
            self.y = nc.dram_tensor("y", [T, D], F32, kind="ExternalOutput").ap()
            din = {}
            shapes = {
                "ffn1_norm": [depth, D], "ffn1_w_gate": [depth, D, F], "ffn1_w_up": [depth, D, F],
                "ffn1_w_down": [depth, F, D], "mix_norm": [depth, D], "w_in": [depth, D, PW],
                "w_a2": [depth, 16, 256], "b_a": [depth, 256], "gla_out_norm": [depth, 128],
                "conv_w": [depth, 3, 512], "w_out": [depth, D, D], "ffn2_norm": [depth, D],
                "ffn2_w_gate": [depth, D, F], "ffn2_w_up": [depth, D, F], "ffn2_w_down": [depth, F, D],
                "final_norm": [D],
            }
            for k, s in shapes.items():
                din[k] = self.dram_in(k, s)
            self.din = din
            big = ["ffn1_w_gate", "ffn1_w_up", "ffn1_w_down", "w_in", "w_out",
                   "ffn2_w_gate", "ffn2_w_up", "ffn2_w_down"]
            self.wbf = {k: nc.dram_tensor("bf_" + k, shapes[k], BF16, kind="Internal").ap() for k in big}

            self.xT = self.sb("xT", [P, DC, TT], F32)
            self.hT = self.sb("hT", [P, DC, TT], BF16)
            self.wb = self.sb("wb", [P, 12 * D], BF16)
            self.ring = self.sb("ring", [P, RING, 4096], BF16)
            self.wa = self.sb("wa", [P, DC, 16], BF16)
            self.rstd = self.sb("rstd", [P, TT], F32)
            self.sq = self.sb("sq", [P, 3, SUB], BF16)
            self.aaug = self.sb("aaug", [32, TT], BF16)
            self.dec = self.sb("dec", [P, 2, NCH], F32)
            self.S32 = self.sb("S32", [P, 2 * depth, 2, 128], F32)
            self.tails = self.sb("tails", [P, depth, 4, 2], F32)
            self.NG = 3 * depth * DC + DC + depth
            self.NCW = depth * 3 * 4
            assert self.NG <= P and self.NCW <= P
            self.prow = self.sb("prow", [P, P], F32)
            self.crow = self.sb("crow", [P, P], F32)
            self.gall = self.sb("gall", [P, self.NG], F32)
            self.cwall = self.sb("cwall", [P, self.NCW], F32)
            n8 = depth * DC
            self.gn1 = self.gall[:, 0:n8].rearrange("p (l c) -> p l c", l=depth)
            self.gnm = self.gall[:, n8:2 * n8].rearrange("p (l c) -> p l c", l=depth)
            self.gn2 = self.gall[:, 2 * n8:3 * n8].rearrange("p (l c) -> p l c", l=depth)
            self.gnf = self.gall[:, 3 * n8:3 * n8 + DC]
            self.gon = self.gall[:, 3 * n8 + DC:3 * n8 + DC + depth]
            self.cw = self.cwall[:, :].rearrange("p (l k c) -> p l k c", l=depth, k=3)
            self.wa2 = self.sb("wa2", [32, depth, 256], BF16)
            self.onesD = self.sb("onesD", [P, P], BF16)
            self.onesV = self.sb("onesV", [P, P], BF16)
            self.ident = self.sb("ident", [P, P], F32)
            self.triA = self.sb("triA", [P, P], F32)
            self.triB = self.sb("triB", [P, P], F32)
            self.mask4 = self.sb("mask4", [P, 4, P], F32)
            ARENA_BYTES = 86 * 1024
            self.arena = self.sb("arena", [P, ARENA_BYTES // 2], BF16)

            def carve(layout):
                off = 0
                for name, shape, dt, parts in layout:
                    n = 1
                    for d_ in shape:
                        n *= d_
                    nb = n * (4 if dt == F32 else 2)
                    v = self.arena[0:parts, off // 2:(off + nb) // 2]
                    if dt == F32:
                        v = v.bitcast(F32)
                    if len(shape) == 2:
                        v = v.rearrange("p (a b) -> p a b", a=shape[0])
                    elif len(shape) == 3:
                        v = v.rearrange("p (a b c) -> p a b c", a=shape[0], b=shape[1])
                    setattr(self, name, v)
                    off += (nb + 63) // 64 * 64
                assert off <= ARENA_BYTES, off
            carve([("act", (12, TT), BF16, P), ("tmpf", (3, SUB), F32, P)])
            carve([("stage", (2, D), F32, P), ("ostage", (2, D), F32, P), ("yT", (DC, P), F32, P),
                   ("wa2f", (depth, 256), F32, 32)])
            carve([("qt", (4, TT), BF16, P), ("kt", (2, TT), BF16, P), ("vtok", (NCH, 512), BF16, P),
                   ("khat", (NCH, 256), BF16, P), ("sg", (4, TT), BF16, P), ("mixT", (DC, TT), BF16, P),
                   ("Lb", (2, 512), F32, P), ("eb", (2, SUB), F32, P), ("enb", (2, SUB), F32, P),
                   ("ebd", (2, SUB), F32, P), ("AT", (2, 512), BF16, P), ("sqo", (2, 512), BF16, P),
                   ("rso", (2, 512), F32, P), ("cu", (2, 2 + SUB), F32, P), ("cacc", (2, SUB), F32, P),
                   ("Sbf", (NCH, 2, 128), BF16, P)])
            self.ps = [es.enter_context(nc.psum_tensor("ps%d" % i, [P, 512], F32)) for i in range(8)]

            self.prologue()
            self.param_transposes()
            for ti in range(self.ntiles):
                phases = []
                for l in range(depth):
                    phases += [("f0", l, (self.gn1, l, False)), ("mx", l, (self.gnm, l, False)), ("f1", l, (self.gn2, l, False))]
                phases.append(("fin", None, (self.gnf, None, True)))
                self.load_tile(ti)
                self.norm_s(phases[0][2], 0)
                self.defer(lambda spec=phases[0][2]: self.norm_s(spec, 1))
                for i, (kind, l, spec) in enumerate(phases[:-1]):
                    nxt = phases[i + 1][2]
                    if kind == "f0":
                        self.ffn(l, 0, ti, nxt)
                    elif kind == "mx":
                        self.mixer(l, ti, nxt)
                    else:
                        self.ffn(l, 1, ti, nxt)
                self.store_tile(ti)
            self.tr.final_wait("act")
            self.sbuf_left = nc.sbuf_bytes_remaining
            self.tr.emit(nc, es)
        return nc

    def prologue(self):
        depth = self.depth
        din = self.din
        self.pending = []
        for l in range(depth):
            for gname in ("f0", "mx", "f1"):
                pcs = self.cast_pieces(l, gname)
                if l == 0 and gname == "f0":
                    for pc in pcs:
                        self.issue_cast(pc, None)
                else:
                    self.pending.append(pcs)
        n8 = depth * DC

        def fnp(e, sem):
            e.dma_start(out=self.prow[0:n8, :], in_=din["ffn1_norm"].rearrange("l (c p) -> (l c) p", p=P)).then_inc(sem, 16)
            e.dma_start(out=self.prow[n8:2 * n8, :], in_=din["mix_norm"].rearrange("l (c p) -> (l c) p", p=P)).then_inc(sem, 16)
            e.dma_start(out=self.prow[2 * n8:3 * n8, :], in_=din["ffn2_norm"].rearrange("l (c p) -> (l c) p", p=P)).then_inc(sem, 16)
            e.dma_start(out=self.prow[3 * n8:3 * n8 + DC, :], in_=din["final_norm"].rearrange("(c p) -> c p", p=P)).then_inc(sem, 16)
            e.dma_start(out=self.prow[3 * n8 + DC:self.NG, :], in_=din["gla_out_norm"]).then_inc(sem, 16)
            e.dma_start(out=self.crow[0:self.NCW, :], in_=din["conv_w"].rearrange("l k (c p) -> (l k c) p", p=P)).then_inc(sem, 16)
        self.tr.dma("sp", "par", 6, fnp, W=["prow"])
        self._param_transpose_pending = True
        self.POOL(lambda e: e.memset(self.wa2f[:, :, :], 0.0), W=["wa2f"])

        def fna(e, sem):
            e.dma_start(out=self.wa2f[0:16, :, :], in_=din["w_a2"].rearrange("l r n -> r l n")).then_inc(sem, 16)
            e.dma_start(out=self.wa2f[16:17, :, :], in_=din["b_a"].rearrange("(o l) n -> o l n", o=1)).then_inc(sem, 16)
        self.tr.dma("sp", "par2", 2, fna, W=["wa2f"])
        self.POOL(lambda e: e.tensor_copy(out=self.wa2[:, :, :], in_=self.wa2f[:, :, :]), R=["wa2f"], W=["wa2"])
        self.POOL(lambda e: e.memset(self.onesD[:, :], 1.0 / D), W=["onesD"])
        self.POOL(lambda e: e.memset(self.onesV[:, :], 1.0 / 128.0), W=["onesV"])
        self.POOL(lambda e: e.memset(self.aaug[:, :], 1.0), W=[("aaug", 0), ("aaug", 1)])
        self.POOL(lambda e: e.memset(self.S32[:, :, :, :], 0.0),
                  W=[("S32", bf_, l, a_, b_) for bf_ in range(2) for l in range(depth) for a_ in range(2) for b_ in range(2)])
        self.POOL(lambda e: e.memset(self.tails[:, :, :, :], 0.0), W=[("tails", l) for l in range(depth)])
        self.POOL(lambda e: e.memset(self.ident[:, :], 1.0), W=["ident"])
        self.POOL(lambda e: e.affine_select(out=self.ident[:, :], in_=self.ident[:, :], pattern=[[-1, P]],
                                            compare_op=ALU.is_equal, fill=0.0, base=0, channel_multiplier=1),
                  R=["ident"], W=["ident"])
        self.POOL(lambda e: e.memset(self.triA[:, :], -1.0 / 16.0), W=["triA"])
        self.POOL(lambda e: e.affine_select(out=self.triA[:, :], in_=self.triA[:, :], pattern=[[1, P]],
                                            compare_op=ALU.is_ge, fill=0.0, base=0, channel_multiplier=-1),
                  R=["triA"], W=["triA"])
        self.POOL(lambda e: e.memset(self.triB[:, :], -1.0 / 16.0), W=["triB"])
        self.POOL(lambda e: e.affine_select(out=self.triB[:, :], in_=self.triB[:, :], pattern=[[-1, P]],
                                            compare_op=ALU.is_ge, fill=0.0, base=-1, channel_multiplier=1),
                  R=["triB"], W=["triB"])
        self.POOL(lambda e: e.memset(self.mask4[:, :, :], 1.0), W=["mask4"])
        self.POOL(lambda e: e.affine_select(out=self.mask4[:, :, :], in_=self.mask4[:, :, :], pattern=[[0, 4], [1, P]],
                                            compare_op=ALU.is_ge, fill=0.0, base=0, channel_multiplier=-1),
                  R=["mask4"], W=["mask4"])

    def cast_pieces(self, l, gname):
        din, wbf = self.din, self.wbf
        out = []
        if gname in ("f0", "f1"):
            pre = "ffn1" if gname == "f0" else "ffn2"
            for b in range(4):
                c0, c1 = b * 704, (b + 1) * 704
                out.append((("wbf", l, gname, "gu%d" % b),
                            [(wbf[pre + "_w_gate"][l][:, c0:c1], din[pre + "_w_gate"][l][:, c0:c1]),
                             (wbf[pre + "_w_up"][l][:, c0:c1], din[pre + "_w_up"][l][:, c0:c1])]))
                if b == 2:
                    out.append((("wbf", l, gname, "d0"),
                                [(wbf[pre + "_w_down"][l][0:1536, :], din[pre + "_w_down"][l][0:1536, :])]))
            out.append((("wbf", l, gname, "d1"),
                        [(wbf[pre + "_w_down"][l][1536:F, :], din[pre + "_w_down"][l][1536:F, :])]))
        else:
            for nm, c0, c1 in (("a", 1536, 1552), ("v", 512, 1024), ("qk", 0, 512), ("g", 1024, 1536),
                               ("u", 1552, 2064), ("b", 2064, 2576), ("c", 2576, 3088)):
                out.append((("wbf", l, gname, nm), [(wbf["w_in"][l][:, c0:c1], din["w_in"][l][:, c0:c1])]))
            out.append((("wbf", l, gname, "o"), [(wbf["w_out"][l], din["w_out"][l])]))
        return out

    def wkey(self, key):
        return key if (key[1], key[2]) == (0, "f0") else key[:3]

    def issue_cast(self, piece, tickkey):
        key, pairs = piece
        if (key[1], key[2]) == (0, "f0"):
            sem = "c0_%d" % self.cast_i
            self.cast_i += 1
        else:
            sem = "cg%d%s" % (key[1], key[2])
        key = self.wkey(key)

        def fn(e, s, pairs=pairs):
            for dst, src_ in pairs:
                e.dma_start(out=dst, in_=src_).then_inc(s, 16)
        self.tr.dma("pool", sem, len(pairs), fn, R=([tickkey] if tickkey is not None else []), W=[key])

    def begin_phase(self, nticks):
        self.cur_pieces = []
        if self.pending:
            if self.phase_i == 0:
                self.cur_pieces += self.pending.pop(0)
            if self.pending:
                self.cur_pieces += self.pending.pop(0)
        self.per_tick = -(-len(self.cur_pieces) // nticks) if self.cur_pieces else 0
        self.phase_i += 1

    def tick(self, flush=False):
        if not self.cur_pieces:
            return
        key = ("tick", self.tick_i)
        self.tick_i += 1
        self.tr.lastw[key] = ("pe", self.tr.nops["pe"] - 1)
        n = len(self.cur_pieces) if flush else self.per_tick
        for _ in range(min(n, len(self.cur_pieces))):
            self.issue_cast(self.cur_pieces.pop(0), key)

    def param_transposes(self):
        for rows, n, dst in ((self.prow, self.NG, self.gall), (self.crow, self.NCW, self.cwall)):
            bk, ps = self.bank()
            self.mm(ps[:, 0:n], rows[0:n, :], self.ident[0:n, 0:n], True, True, R=["prow", "ident"], W=[bk])
            self.ACT(lambda e, ps=ps, n=n, dst=dst: e.activation(out=dst[:, 0:n], in_=ps[:, 0:n], func=AF.Copy),
                     R=[bk], W=["par"])

    def load_tile(self, ti):
        tok0 = ti * TT
        for tc in range(NCH):
            slot = tc % 2
            src = self.x[tok0 + tc * P: tok0 + (tc + 1) * P, :]
            self.tr.dma("sp", "xin%d" % slot, 1,
                        lambda e, sem, slot=slot, src=src: e.dma_start(out=self.stage[:, slot, :], in_=src).then_inc(sem, 16),
                        W=[("stage", slot)])
            for g in range(2):
                bk, ps = self.bank()
                for j in range(4):
                    dc = 4 * g + j
                    self.PE(lambda e, ps=ps, j=j, dc=dc, slot=slot: e.transpose(
                        ps[:, j * P:(j + 1) * P], self.stage[:, slot, dc * P:(dc + 1) * P], self.ident[:, :]),
                        R=[("stage", slot), "ident"], W=[bk])
                self.ACT(lambda e, ps=ps, g=g, tc=tc: e.activation(
                    out=self.xT[:, 4 * g:4 * g + 4, tc * P:(tc + 1) * P],
                    in_=ps[:, :].rearrange("p (a b) -> p a b", a=4), func=AF.Copy),
                    R=[bk], W=[("xT", dc, tc // 4) for dc in range(4 * g, 4 * g + 4)])

    def store_tile(self, ti):
        tok0 = ti * TT
        self.flush_deferred()
        self.phase("io")
        for tc in range(NCH):
            s = tc // 4
            slot = tc % 2
            for dc in range(DC):
                eng = self.DVE
                eng(lambda e, dc=dc, tc=tc: e.scalar_tensor_tensor(
                    out=self.yT[:, dc, :], in0=self.xT[:, dc, tc * P:(tc + 1) * P], scalar=self.gnf[:, dc:dc + 1],
                    in1=self.rstd[:, tc * P:(tc + 1) * P], op0=ALU.mult, op1=ALU.mult),
                    R=[("xT", dc, s), ("rstd", s), "par"], W=[("yT", dc)])
            for g in range(2):
                bk, ps = self.bank()
                for j in range(4):
                    dc = 4 * g + j
                    self.PE(lambda e, ps=ps, j=j, dc=dc: e.transpose(
                        ps[:, j * P:(j + 1) * P], self.yT[:, dc, :], self.ident[:, :]),
                        R=[("yT", dc), "ident"], W=[bk])
                self.ACT(lambda e, ps=ps, g=g, slot=slot: e.activation(
                    out=self.ostage[:, slot, g * 512:(g + 1) * 512], in_=ps[:, :], func=AF.Copy),
                    R=[bk], W=[("ostage", slot)])
            dst = self.y[tok0 + tc * P: tok0 + (tc + 1) * P, :]
            self.tr.dma("act", "out%d" % slot, 1,
                        lambda e, sem, slot=slot, dst=dst: e.dma_start(out=dst, in_=self.ostage[:, slot, :]).then_inc(sem, 16),
                        R=[("ostage", slot)])

    def norm_s(self, spec, s):
        gain, l, final = spec
        cs = slice(s * SUB, (s + 1) * SUB)
        bk, ps = self.bank()
        for dc in range(DC):
            q = self.uid % 3
            self.uid += 1
            self.ACT(lambda e, dc=dc, q=q, cs=cs: e.activation(out=self.sq[:, q, :], in_=self.xT[:, dc, cs], func=AF.Square),
                     R=[("xT", dc, s)], W=[("sq", q)])
            self.mm(ps[:, :], self.onesD[:, :], self.sq[:, q, :], dc == 0, dc == DC - 1,
                    R=[("sq", q), "onesD"], W=[bk])
        self.ACT(lambda e, ps=ps, cs=cs: e.activation(out=self.rstd[:, cs], in_=ps[:, :], func=AF.Ln, bias=EPS),
                 R=[bk], W=[("rstd", s)])
        self.ACT(lambda e, cs=cs: e.activation(out=self.rstd[:, cs], in_=self.rstd[:, cs], func=AF.Exp, scale=-0.5),
                 R=[("rstd", s)], W=[("rstd", s)])
        if final:
            return
        for dc in range(DC):
            g_ap = gain[:, l, dc:dc + 1]
            self.DVE(lambda e, dc=dc, cs=cs, g_ap=g_ap: e.scalar_tensor_tensor(
                out=self.hT[:, dc, cs], in0=self.xT[:, dc, cs], scalar=g_ap, in1=self.rstd[:, cs],
                op0=ALU.mult, op1=ALU.mult),
                R=[("xT", dc, s), ("rstd", s), "par"], W=[("hT", dc, s)])

    def defer(self, fn):
        self.deferred.append(fn)

    def flush_deferred(self):
        d, self.deferred = self.deferred, []
        for fn in d:
            fn()

    def residual_tail(self, emit_group, nxt):
        for s in range(NS):
            for dc in range(DC):
                emit_group(s, dc)
                if s == 1 and dc == 1:
                    self.norm_s(nxt, 0)
        self.defer(lambda: self.norm_s(nxt, 1))

    def slab(self, parts, R):
        k = self.slab_i
        self.slab_i += 1
        slot = k % RING
        sl = self.ring[:, slot, :]

        def fn(e, sem, parts=parts, sl=sl):
            for dstf, src in parts:
                e.dma_start(out=dstf(sl), in_=src).then_inc(sem, 16)
        self.tr.dma("sp", "ring%d" % slot, len(parts), fn, R=R, W=[("ring", slot)])
        return ("ring", slot), sl

    def load_wb(self, view_fn, src, R):
        def fn(e, sem):
            e.dma_start(out=view_fn(self.wb[:, :]), in_=src).then_inc(sem, 16)
        self.tr.dma("act", "wb", 1, fn, R=R, W=["wb"])

    def ffn(self, l, which, ti, nxt):
        pre = "ffn1" if which == 0 else "ffn2"
        gname = "f0" if which == 0 else "f1"
        self.begin_phase(13)
        wg = self.wbf[pre + "_w_gate"][l]
        wu = self.wbf[pre + "_w_up"][l]
        wd = self.wbf[pre + "_w_down"][l]
        gain = self.gn1 if which == 0 else self.gn2
        self.phase("ffn")
        slab0 = 0
        for half, nsl in enumerate(HALF_SLABS):
            nfc = nsl * 2
            fc0 = slab0 * 2
            self.load_wb(lambda w, nfc=nfc: w[:, 0:nfc * D].rearrange("p (j d) -> p j d", j=nfc),
                         wd[fc0 * P:(fc0 + nfc) * P, :].rearrange("(j p) d -> p j d", p=P), R=[self.wkey(("wbf", l, gname, "d%d" % half))])
            def load_gu(si, slab0=slab0):
                col0 = (slab0 + si) * FS
                rk, sl = self.slab([
                    (lambda s_: s_[:, 0:2048].rearrange("p (k c) -> p k c", k=DC), wg[:, col0:col0 + FS].rearrange("(k p) c -> p k c", p=P)),
                    (lambda s_: s_[:, 2048:4096].rearrange("p (k c) -> p k c", k=DC), wu[:, col0:col0 + FS].rearrange("(k p) c -> p k c", p=P)),
                ], R=[self.wkey(("wbf", l, gname, "gu%d" % b)) for b in sorted({col0 // 704, (col0 + FS - 1) // 704})])
                gv = sl[:, 0:2048].rearrange("p (k c) -> p k c", k=DC)
                uv = sl[:, 2048:4096].rearrange("p (k c) -> p k c", k=DC)
                return rk, gv, uv

            def gu_item(slabinfo, si, c, s):
                rk, gv, uv = slabinfo
                fcl = si * 2 + c
                cs = slice(s * SUB, (s + 1) * SUB)
                bg, pg = self.bank()
                bu, pu = self.bank()
                for kc in range(DC):
                    self.mm(pg[:, :], gv[:, kc, c * P:(c + 1) * P], self.hT[:, kc, cs], kc == 0, kc == DC - 1,
                            R=[rk, ("hT", kc, s)], W=[bg])
                for kc in range(DC):
                    self.mm(pu[:, :], uv[:, kc, c * P:(c + 1) * P], self.hT[:, kc, cs], kc == 0, kc == DC - 1,
                            R=[rk, ("hT", kc, s)], W=[bu])
                q = self.uid % 3
                self.uid += 1
                self.ACT(lambda e, pg=pg, q=q: e.activation(out=self.tmpf[:, q, :], in_=pg[:, :], func=AF.Silu),
                         R=[bg], W=[("tmpf", q)])
                self.DVE(lambda e, pu=pu, q=q, fcl=fcl, cs=cs: e.tensor_tensor(
                    out=self.act[:, fcl, cs], in0=pu[:, :], in1=self.tmpf[:, q, :], op=ALU.mult),
                    R=[bu, ("tmpf", q)], W=[("act", fcl, s)])

            si0 = 0
            if half == 0:
                sA, sB = load_gu(0), load_gu(1)
                gu_item(sA, 0, 0, 0)
                gu_item(sA, 0, 1, 0)
                self.flush_deferred()
                gu_item(sB, 1, 0, 0)
                gu_item(sB, 1, 1, 0)
                self.tick()
                gu_item(sA, 0, 0, 1)
                gu_item(sA, 0, 1, 1)
                gu_item(sB, 1, 0, 1)
                gu_item(sB, 1, 1, 1)
                self.tick()
                si0 = 2
            for si in range(si0, nsl):
                info = load_gu(si)
                for c in range(2):
                    for s in range(NS):
                        gu_item(info, si, c, s)
                self.tick()
            wv = self.wb[:, 0:nfc * D].rearrange("p (j d) -> p j d", j=nfc)

            def down_group(s, dc, wv=wv, nfc=nfc):
                cs = slice(s * SUB, (s + 1) * SUB)
                bk, ps = self.bank()
                for j in range(nfc):
                    self.mm(ps[:, :], wv[:, j, dc * P:(dc + 1) * P], self.act[:, j, cs], j == 0, j == nfc - 1,
                            R=["wb", ("act", j, s)], W=[bk])
                self.DVE(lambda e, ps=ps, dc=dc, cs=cs: e.scalar_tensor_tensor(
                    out=self.xT[:, dc, cs], in0=ps[:, :], scalar=0.5, in1=self.xT[:, dc, cs],
                    op0=ALU.mult, op1=ALU.add),
                    R=[bk, ("xT", dc, s)], W=[("xT", dc, s)])
            if half == 0:
                for s in range(NS):
                    for dc in range(DC):
                        down_group(s, dc)
            else:
                self.residual_tail(down_group, nxt)
            self.tick(flush=(half == 1))
            slab0 += nsl

    def mixer(self, l, ti, nxt):
        def grp(*names):
            return [self.wkey(("wbf", l, "mx", nm)) for nm in names]
        self.begin_phase(16)
        win = self.wbf["w_in"][l]
        wout = self.wbf["w_out"][l]
        self.phase("mix")
        self.POOL(lambda e: e.memset(self.qt[:, :, :], 0.0), W=[("qt", kc2, s) for kc2 in range(2) for s in range(NS)])

        def wcols(c0, n):
            return win[:, c0:c0 + n].rearrange("(k p) c -> p k c", p=P)

        def fa(e, sem):
            e.dma_start(out=self.wa[:, :, :], in_=wcols(1536, 16)).then_inc(sem, 16)
        self.tr.dma("sp", "wa", 1, fa, R=grp("a"), W=["wa"])
        def alr(s):
            cs = slice(s * SUB, (s + 1) * SUB)
            bk, ps = self.bank()
            for kc in range(DC):
                self.mm(ps[0:16, :], self.wa[:, kc, :], self.hT[:, kc, cs], kc == 0, kc == DC - 1,
                        R=["wa", ("hT", kc, s)], W=[bk])
            self.ACT(lambda e, ps=ps, cs=cs: e.activation(out=self.aaug[0:16, cs], in_=ps[0:16, :], func=AF.Copy),
                     R=[bk], W=[("aaug", s)])
        alr(0)
        rv, slv = self.slab([(lambda s_: s_[:, :].rearrange("p (k c) -> p k c", k=DC), wcols(512, 512))], R=grp("v"))
        vv = slv[:, :].rearrange("p (k c) -> p k c", k=DC)
        rqk, slqk = self.slab([(lambda s_: s_[:, :].rearrange("p (k c) -> p k c", k=DC), wcols(0, 512))], R=grp("qk"))
        qkv = slqk[:, :].rearrange("p (k c) -> p k c", k=DC)

        def vproj(n):
            bk, ps = self.bank()
            for kc in range(DC):
                self.mm(ps[:, :], self.hT[:, kc, n * P:(n + 1) * P], vv[:, kc, :], kc == 0, kc == DC - 1,
                        R=[rv, ("hT", kc, n // 4)], W=[bk])
            self.ACT(lambda e, ps=ps, n=n: e.activation(out=self.vtok[:, n, :], in_=ps[:, :], func=AF.Copy),
                     R=[bk], W=[("vtok", n)])

        bt_banks = {}
        for pr in range(NCH // 2):
            s = pr // 2
            if pr == 1:
                self.flush_deferred()
            if pr == 2:
                alr(1)
            if pr % 2 == 0:
                for kc2 in range(2):
                    bt_banks[(s, kc2)] = self.bank(hold=True)
            bkz, psz = self.bank()
            for j in range(2):
                n = 2 * pr + j
                self.mm(psz[:, j * 256:(j + 1) * 256], self.aaug[:, n * P:(n + 1) * P], self.wa2[:, l, :], True, True,
                        R=[("aaug", s), "wa2"], W=[bkz])
            lq = pr % 2
            self.ACT(lambda e, psz=psz, lq=lq: e.activation(out=self.Lb[:, lq, :], in_=psz[:, :], func=AF.Exp, scale=-1.0),
                     R=[bkz], W=[("Lb", lq)])
            self.ACT(lambda e, lq=lq: e.activation(out=self.Lb[:, lq, :], in_=self.Lb[:, lq, :], func=AF.Ln, bias=1.0),
                     R=[("Lb", lq)], W=[("Lb", lq)])
            vproj(2 * pr)
            vproj(2 * pr + 1)
            bkd, psd = self.bank()
            for j in range(2):
                n = 2 * pr + j
                nl = n % 4
                Lv = self.Lb[:, lq, j * 256:(j + 1) * 256]
                for kc2 in range(2):
                    bkt, pst = bt_banks[(s, kc2)]
                    self.mm(pst[:, nl * P:(nl + 1) * P], Lv[:, kc2 * P:(kc2 + 1) * P], self.triA[:, :], True, True,
                            R=[("Lb", lq), "triA"], W=[bkt])
                self.mm(psd[:, j * 256:(j + 1) * 256], self.triB[:, :], Lv, True, True,
                        R=[("Lb", lq), "triB"], W=[bkd])
            bkk, psk = self.bank()
            for j in range(2):
                n = 2 * pr + j
                for kc in range(DC):
                    self.mm(psk[:, j * 256:(j + 1) * 256], self.hT[:, kc, n * P:(n + 1) * P], qkv[:, kc, 256:512],
                            kc == 0, kc == DC - 1, R=[rqk, ("hT", kc, s)], W=[bkk])
            eq = pr % 2
            self.ACT(lambda e, psd=psd, eq=eq: e.activation(out=self.ebd[:, eq, :], in_=psd[:, :], func=AF.Exp),
                     R=[bkd], W=[("ebd", eq)])
            self.DVE(lambda e, psk=psk, eq=eq, pr=pr: e.tensor_tensor(
                out=self.khat[:, 2 * pr:2 * pr + 2, :], in0=psk[:, :].rearrange("p (a b) -> p a b", a=2),
                in1=self.ebd[:, eq, :].rearrange("p (a b) -> p a b", a=2), op=ALU.mult),
                R=[bkk, ("ebd", eq)], W=[("khat", 2 * pr), ("khat", 2 * pr + 1)])
            if pr % 2 == 1:
                cs = slice(s * SUB, (s + 1) * SUB)
                for kc2 in range(2):
                    bkt, pst = bt_banks[(s, kc2)]
                    self.ACT(lambda e, pst=pst, kc2=kc2: e.activation(out=self.eb[:, kc2, :], in_=pst[:, :], func=AF.Exp),
                             R=[bkt], W=[("eb", kc2)])
                    self.ACT(lambda e, pst=pst, kc2=kc2: e.activation(out=self.enb[:, kc2, :], in_=pst[:, :], func=AF.Exp, scale=-1.0),
                             R=[bkt], W=[("enb", kc2)])
                    self.DVE(lambda e, kc2=kc2, s=s: e.tensor_copy(out=self.dec[:, kc2, 4 * s:4 * s + 4], in_=self.eb[:, kc2, 127:512:128]),
                             R=[("eb", kc2)], W=[("dec", kc2, s)])
                    bq, pq = self.bank()
                    for kc in range(DC):
                        self.mm(pq[:, :], qkv[:, kc, kc2 * P:(kc2 + 1) * P], self.hT[:, kc, cs], kc == 0, kc == DC - 1,
                                R=[rqk, ("hT", kc, s)], W=[bq])
                    for hl in range(2):
                        pr_ = slice(hl * 64, (hl + 1) * 64)
                        self.DVE(lambda e, pq=pq, kc2=kc2, cs=cs, pr_=pr_, hl=hl: e.scalar_tensor_tensor(
                            out=self.qt[pr_, 2 * kc2 + hl, cs], in0=pq[pr_, :], scalar=0.125, in1=self.eb[pr_, kc2, :],
                            op0=ALU.mult, op1=ALU.mult),
                            R=[bq, ("eb", kc2)], W=[("qt", kc2, s)])
                    bk_, pk = self.bank()
                    for kc in range(DC):
                        self.mm(pk[:, :], qkv[:, kc, 256 + kc2 * P:256 + (kc2 + 1) * P], self.hT[:, kc, cs], kc == 0, kc == DC - 1,
                                R=[rqk, ("hT", kc, s)], W=[bk_])
                    self.DVE(lambda e, pk=pk, kc2=kc2, cs=cs: e.tensor_tensor(
                        out=self.kt[:, kc2, cs], in0=pk[:, :], in1=self.enb[:, kc2, :], op=ALU.mult),
                        R=[bk_, ("enb", kc2)], W=[("kt", kc2, s)])
                    self.held.discard(bkt[1])
            self.tick()
        rg, slg = self.slab([(lambda s_: s_[:, :].rearrange("p (k c) -> p k c", k=DC), wcols(1024, 512))], R=grp("g"))
        gvw = slg[:, :].rearrange("p (k c) -> p k c", k=DC)
        for n in range(NCH):
            s = n // 4
            bku, psu = self.bank()
            for kc2 in range(2):
                self.mm(psu[:, kc2 * 256:(kc2 + 1) * 256], self.khat[:, n, kc2 * P:(kc2 + 1) * P],
                        self.vtok[:, n, kc2 * 256:(kc2 + 1) * 256], True, True,
                        R=[("khat", n), ("vtok", n)], W=[bku])
            cur, nxb = n % 2, (n + 1) % 2
            ic, io = cur * self.depth + l, nxb * self.depth + l
            self.POOL(lambda e, n=n, ic=ic: e.tensor_copy(out=self.Sbf[:, n, :, :], in_=self.S32[:, ic, :, :]),
                      R=[("S32", cur, l, a_, b_) for a_ in range(2) for b_ in range(2)], W=[("Sbf", n)])
            for kc2 in range(2):
                for hl in range(2):
                    pr_ = slice(hl * 64, (hl + 1) * 64)
                    c0 = kc2 * 256 + hl * 128
                    self.DVE(lambda e, psu=psu, kc2=kc2, pr_=pr_, c0=c0, n=n, ic=ic, io=io: e.scalar_tensor_tensor(
                        out=self.S32[pr_, io, kc2, :], in0=self.S32[pr_, ic, kc2, :], scalar=self.dec[pr_, kc2, n:n + 1],
                        in1=psu[pr_, c0:c0 + 128], op0=ALU.mult, op1=ALU.add),
                        R=[bku, ("S32", cur, l, kc2, hl), ("dec", kc2, s)], W=[("S32", nxb, l, kc2, hl)])
            sg_s, sg_c = n // 4, n % 4
            cs = slice(sg_s * SUB, (sg_s + 1) * SUB)
            bk, ps = self.bank()
            for kc in range(DC):
                self.mm(ps[:, :], gvw[:, kc, sg_c * P:(sg_c + 1) * P], self.hT[:, kc, cs], kc == 0, kc == DC - 1,
                        R=[rg, ("hT", kc, sg_s)], W=[bk])
            self.ACT(lambda e, ps=ps, sg_c=sg_c, cs=cs: e.activation(out=self.sg[:, sg_c, cs], in_=ps[:, :], func=AF.Silu),
                     R=[bk], W=[("sg", sg_c, sg_s)])
        self.tick()
        self.load_wb(lambda w: w[:, 0:DC * D].rearrange("p (k d) -> p k d", k=DC),
                     wout.rearrange("(k p) d -> p k d", p=P), R=grp("o"))
        wov = self.wb[:, 0:DC * D].rearrange("p (k d) -> p k d", k=DC)

        def conv_item(c, s):
            rc, slc = self.slab([
                (lambda s_, j=j: s_[:, j * 1024:(j + 1) * 1024].rearrange("p (k c) -> p k c", k=DC),
                 wcols(1552 + 512 * j + c * P, P)) for j in range(3)], R=grp("u", "b", "c"))
            cs = slice(s * SUB, (s + 1) * SUB)
            pss = []
            for j in range(3):
                wvj = slc[:, j * 1024:(j + 1) * 1024].rearrange("p (k c) -> p k c", k=DC)
                bk, ps = self.bank()
                for kc in range(DC):
                    self.mm(ps[:, :], wvj[:, kc, :], self.hT[:, kc, cs], kc == 0, kc == DC - 1,
                            R=[rc, ("hT", kc, s)], W=[bk])
                pss.append((bk, ps))
            (bu, pu), (bb, pb), (bc, pc) = pss
            q = self.uid % 2
            self.uid += 1
            self.ACT(lambda e, pc=pc, q=q: e.activation(out=self.cacc[:, q, :], in_=pc[:, :], func=AF.Copy),
                     R=[bc], W=[("cacc", q)])
            self.ACT(lambda e, pb=pb, q=q: e.activation(out=self.ebd[:, q, :], in_=pb[:, :], func=AF.Copy),
                     R=[bb], W=[("ebd", q)])
            self.POOL(lambda e, q=q, c=c: e.tensor_copy(out=self.cu[:, q, 0:2], in_=self.tails[:, l, c, :]),
                      R=[("tails", l, c)], W=[("cu", q)])
            self.DVE(lambda e, pu=pu, q=q: e.tensor_tensor(out=self.cu[:, q, 2:2 + SUB], in0=pu[:, :], in1=self.cacc[:, q, :], op=ALU.mult),
                     R=[bu, ("cacc", q), ("cu", q)], W=[("cu", q)])
            self.POOL(lambda e, q=q, c=c: e.tensor_copy(out=self.tails[:, l, c, :], in_=self.cu[:, q, SUB:SUB + 2]),
                      R=[("cu", q)], W=[("tails", l, c)])
            self.ACT(lambda e, q=q, c=c: e.activation(out=self.cacc[:, q, :], in_=self.cu[:, q, 2:2 + SUB], func=AF.Copy,
                                                      scale=self.cw[:, l, 2, c:c + 1]),
                     R=[("cu", q), "par"], W=[("cacc", q)])
            self.DVE(lambda e, q=q, c=c: e.scalar_tensor_tensor(out=self.cacc[:, q, :], in0=self.cu[:, q, 1:1 + SUB],
                                                                scalar=self.cw[:, l, 1, c:c + 1], in1=self.cacc[:, q, :],
                                                                op0=ALU.mult, op1=ALU.add),
                     R=[("cu", q), ("cacc", q), "par"], W=[("cacc", q)])
            self.DVE(lambda e, q=q, c=c: e.scalar_tensor_tensor(out=self.cacc[:, q, :], in0=self.cu[:, q, 0:SUB],
                                                                scalar=self.cw[:, l, 0, c:c + 1], in1=self.cacc[:, q, :],
                                                                op0=ALU.mult, op1=ALU.add),
                     R=[("cu", q), ("cacc", q), "par"], W=[("cacc", q)])
            self.DVE(lambda e, q=q, c=c, cs=cs: e.tensor_tensor(out=self.mixT[:, 4 + c, cs], in0=self.ebd[:, q, :], in1=self.cacc[:, q, :], op=ALU.mult),
                     R=[("ebd", q), ("cacc", q)], W=[("mixT", 4 + c, s)])

        def gla_scores(n):
            s = n // 4
            cn = slice(n * P, (n + 1) * P)
            bsc, psc = self.bank()
            for h in range(4):
                kc2 = h // 2
                self.mm(psc[:, h * P:(h + 1) * P], self.kt[:, kc2, cn], self.qt[:, h, cn], True, True,
                        R=[("kt", kc2, s), ("qt", kc2, s)], W=[bsc])
            q = n % 2
            self.DVE(lambda e, psc=psc, q=q: e.tensor_tensor(
                out=self.AT[:, q, :], in0=psc[:, :], in1=self.mask4[:, :, :].rearrange("p a b -> p (a b)"), op=ALU.mult),
                R=[bsc, "mask4"], W=[("AT", q)])

        def gla_out(n):
            s = n // 4
            cn = slice(n * P, (n + 1) * P)
            q = n % 2
            bo, po = self.bank()
            for h in range(4):
                kc2 = h // 2
                self.mm(po[:, h * P:(h + 1) * P], self.vtok[:, n, h * P:(h + 1) * P], self.AT[:, q, h * P:(h + 1) * P], True, False,
                        R=[("vtok", n), ("AT", q)], W=[bo])
                self.mm(po[:, h * P:(h + 1) * P], self.Sbf[:, n, kc2, :], self.qt[:, h, cn], False, True,
                        R=[("Sbf", n), ("qt", kc2, s)], W=[bo])
            self.ACT(lambda e, po=po, q=q: e.activation(out=self.sqo[:, q, :], in_=po[:, :], func=AF.Square),
                     R=[bo], W=[("sqo", q)])
            self.ACT(lambda e, po=po, q=q: e.activation(out=self.Lb[:, q, :], in_=po[:, :], func=AF.Copy),
                     R=[bo], W=[("Lb", q)])

        def gla_norm(n):
            s = n // 4
            cn = slice(n * P, (n + 1) * P)
            q = n % 2
            bst, pst = self.bank()
            self.mm(pst[:, :], self.onesV[:, :], self.sqo[:, q, :], True, True, R=[("sqo", q), "onesV"], W=[bst])
            self.ACT(lambda e, pst=pst, q=q: e.activation(out=self.rso[:, q, :], in_=pst[:, :], func=AF.Ln, bias=EPS),
                     R=[bst], W=[("rso", q)])
            self.ACT(lambda e, q=q: e.activation(out=self.rso[:, q, :], in_=self.rso[:, q, :], func=AF.Exp, scale=-0.5),
                     R=[("rso", q)], W=[("rso", q)])
            self.DVE(lambda e, q=q: e.scalar_tensor_tensor(out=self.rso[:, q, :], in0=self.Lb[:, q, :], scalar=self.gon[:, l:l + 1],
                                                           in1=self.rso[:, q, :], op0=ALU.mult, op1=ALU.mult),
                     R=[("Lb", q), ("rso", q), "par"], W=[("rso", q)])
            self.POOL(lambda e, q=q, cn=cn: e.tensor_tensor(
                out=self.mixT[:, 0:4, cn], in0=self.rso[:, q, :].rearrange("p (a b) -> p a b", a=4), in1=self.sg[:, :, cn], op=ALU.mult),
                R=[("rso", q)] + [("sg", c, s) for c in range(4)], W=[("mixT", c, s) for c in range(4)])

        def wout_group(s, dc):
            cs = slice(s * SUB, (s + 1) * SUB)
            bk, ps = self.bank()
            for kc in range(DC):
                self.mm(ps[:, :], wov[:, kc, dc * P:(dc + 1) * P], self.mixT[:, kc, cs], kc == 0, kc == DC - 1,
                        R=["wb", ("mixT", kc, s)], W=[bk])
            self.DVE(lambda e, ps=ps, dc=dc, cs=cs: e.tensor_tensor(out=self.xT[:, dc, cs], in0=ps[:, :], in1=self.xT[:, dc, cs], op=ALU.add),
                     R=[bk, ("xT", dc, s)], W=[("xT", dc, s)])

        gla_scores(0)
        for i in range(NCH + 1):
            if i + 1 < NCH:
                gla_scores(i + 1)
            if i < NCH:
                gla_out(i)
            if i >= 1:
                conv_item((i - 1) % 4, (i - 1) // 4)
                gla_norm(i - 1)
            for dc in {5: (0,), 6: (1,), 7: (2, 3), 8: (4, 5, 6, 7)}.get(i, ()):
                wout_group(0, dc)
            self.tick()
        for dc in range(DC):
            wout_group(1, dc)
            if dc == 1:
                self.norm_s(nxt, 0)
        self.defer(lambda: self.norm_s(nxt, 1))
        self.tick(flush=True)


_W_NAMES = ["ffn1_norm", "ffn1_w_gate", "ffn1_w_up", "ffn1_w_down", "mix_norm", "w_in", "w_a2", "b_a",
            "gla_out_norm", "conv_w", "w_out", "ffn2_norm", "ffn2_w_gate", "ffn2_w_up", "ffn2_w_down", "final_norm"]


def run(inputs, trace=False, stages=("f0", "mx", "f1")):
    x = np.ascontiguousarray(inputs["x"], dtype=np.float32)
    B, T, _ = x.shape
    depth = inputs["w_in"].shape[0]
    nc = Builder(T, depth, stages).build()
    w = {k: np.ascontiguousarray(inputs[k], dtype=np.float32) for k in _W_NAMES}
    in_maps = [dict(w, x=x[b]) for b in range(B)]
    res = run_bass_kernel_spmd(nc, in_maps, core_ids=list(range(B)), trace=trace)
    out = np.stack([res.results[b]["y"] for b in range(B)], axis=0)
    return out, res


def kernel(**inputs):
    out, _ = run(inputs)
    return out
```

```python
import numpy as np
from contextlib import ExitStack
import concourse.bass as bass
import concourse.mybir as mybir
from concourse.bass_utils import run_bass_kernel_spmd

F32 = mybir.dt.float32
BF16 = mybir.dt.bfloat16
AF = mybir.ActivationFunctionType
ALU = mybir.AluOpType

P = 128
D = 1024
DC = D // P
F = 2816
FS = 256
NFS = F // FS
HALF_SLABS = (6, 5)
PW = 3088
TT = 1024
SUB = 512
NS = TT // SUB
NCH = TT // P
EPS = 1e-6
RING = 3
COMPUTE = ("pe", "act", "dve", "pool")
ARENA_NAMES = {"act", "tmpf", "stage", "ostage", "yT", "wa2f", "qt", "kt", "vtok", "khat", "sg", "mixT",
               "Lb", "eb", "enb", "ebd", "AT", "sqo", "rso", "cu", "cacc", "Sbf"}


def is_arena(key):
    return (key[0] if isinstance(key, tuple) else key) in ARENA_NAMES


class Tracker:
    def __init__(self):
        self.streams = {k: [] for k in ("pe", "act", "dve", "pool", "sp")}
        self.nops = {k: 0 for k in COMPUTE}
        self.dma_cnt = {}
        self.lastw = {}
        self.readers = {}
        self.signal = {k: set() for k in COMPUTE}
        self.floor = {}

    def arena_switch(self):
        for table in (self.lastw, self.readers):
            for key in [k for k in table if is_arena(k)]:
                v = table.pop(key)
                items = [v] if table is self.lastw else list(v.items())
                for s, i in items:
                    if self.floor.get(s, -1) < i:
                        self.floor[s] = i

    def _deps(self, me, R, W):
        deps = {}

        def add(src, idx):
            if (src, idx) == me:
                return
            if src == "pe" and me[0] == "pe":
                return
            if deps.get(src, -1) < idx:
                deps[src] = idx

        if self.floor and any(is_arena(r) for r in list(R) + list(W)):
            for s, i in self.floor.items():
                add(s, i)
        for r in R:
            w = self.lastw.get(r)
            if w is not None:
                add(*w)
        for r in W:
            w = self.lastw.get(r)
            if w is not None:
                add(*w)
            rd = self.readers.get(r)
            if rd:
                for s, i in rd.items():
                    add(s, i)
        for r in R:
            rd = self.readers.setdefault(r, {})
            if rd.get(me[0], -1) < me[1]:
                rd[me[0]] = me[1]
        for r in W:
            self.lastw[r] = me
            self.readers[r] = {}
        for s, i in deps.items():
            if s in self.signal:
                self.signal[s].add(i)
        return deps

    def op(self, eng, fn, R=(), W=()):
        idx = self.nops[eng]
        self.nops[eng] += 1
        deps = self._deps((eng, idx), R, W)
        self.streams[eng].append(("op", idx, fn, deps))

    def dma(self, queue, key, n, fn, R=(), W=()):
        cnt = self.dma_cnt.get(key, 0) + n
        self.dma_cnt[key] = cnt
        deps = self._deps(("dma:" + key, cnt), R, W)
        self.streams[queue].append(("dma", key, fn, deps))

    def final_wait(self, stream):
        deps = {"dma:" + k: c for k, c in self.dma_cnt.items() if k.startswith("out")}
        self.streams[stream].append(("wait", None, None, deps))

    def emit(self, nc, es):
        sems = {}
        for e in COMPUTE:
            sems[e] = es.enter_context(nc.semaphore("s_" + e))
        for k in self.dma_cnt:
            sems["dma:" + k] = es.enter_context(nc.semaphore("d_" + k))
        rank = {}
        for e in COMPUTE:
            rank[e] = {idx: r + 1 for r, idx in enumerate(sorted(self.signal[e]))}

        def run(stream, eng):
            waited = {}
            for kind, a, fn, deps in self.streams[stream]:
                for s, i in deps.items():
                    val = rank[s][i] if s in rank else 16 * i
                    if waited.get(s, 0) >= val:
                        continue
                    waited[s] = val
                    eng.wait_ge(sems[s], val)
                if kind == "op":
                    ins = fn(eng)
                    if a in rank[stream]:
                        ins.then_inc(sems[stream], 1)
                elif kind == "dma":
                    fn(eng, sems["dma:" + a])

        block = es.enter_context(nc.Block())

        @block.tensor
        def _(e):
            run("pe", e)

        @block.scalar
        def _(e):
            run("act", e)

        @block.vector
        def _(e):
            run("dve", e)

        @block.gpsimd
        def _(e):
            run("pool", e)

        @block.sync
        def _(e):
            run("sp", e)


class Builder:
    def __init__(self, T, depth, stages=("f0", "mx", "f1")):
        self.stages = stages
        self.T = T
        self.depth = depth
        self.ntiles = T // TT
        self.nc = bass.Bass("TRN2", target_bir_lowering=False)
        self.tr = Tracker()
        self.bank_i = 0
        self.held = set()
        self.cur_phase = "io"
        self.slab_i = 0
        self.uid = 0
        self.cast_i = 0
        self.tick_i = 0
        self.phase_i = 0
        self.cur_pieces = []
        self.deferred = []

    def dram_in(self, name, shape):
        return self.nc.dram_tensor(name, list(shape), F32, kind="ExternalInput").ap()

    def sb(self, name, shape, dt):
        return self.es.enter_context(self.nc.sbuf_tensor(name, list(shape), dt))

    def bank(self, hold=False):
        i = self.bank_i
        while i in self.held:
            i = (i + 1) % 8
        self.bank_i = (i + 1) % 8
        if hold:
            self.held.add(i)
        return ("ps", i), self.ps[i]

    def phase(self, name):
        if self.cur_phase != name:
            self.tr.arena_switch()
            self.cur_phase = name

    def PE(self, fn, R=(), W=()):
        self.tr.op("pe", fn, R, W)

    def ACT(self, fn, R=(), W=()):
        self.tr.op("act", fn, R, W)

    def DVE(self, fn, R=(), W=()):
        self.tr.op("dve", fn, R, W)

    def POOL(self, fn, R=(), W=()):
        self.tr.op("pool", fn, R, W)

    def mm(self, out, lhsT, rhs, start, stop, R, W):
        self.PE(lambda e: e.matmul(out, lhsT, rhs, start=start, stop=stop), R, W)

    def build(self):
        nc = self.nc
        depth = self.depth
        T = self.T
        with ExitStack() as es:
            self.es = es
            es.enter_context(nc.allow_non_contiguous_dma(reason="small param loads"))
            self.x = self.dram_in("x", [T, D])
            self.y = nc.dram_tensor("y", [T, D], F32, kind="ExternalOutput").ap()
            din = {}
            shapes = {
                "ffn1_norm": [depth, D], "ffn1_w_gate": [depth, D, F], "ffn1_w_up": [depth, D, F],
                "ffn1_w_down": [depth, F, D], "mix_norm": [depth, D], "w_in": [depth, D, PW],
                "w_a2": [depth, 16, 256], "b_a": [depth, 256], "gla_out_norm": [depth, 128],
                "conv_w": [depth, 3, 512], "w_out": [depth, D, D], "ffn2_norm": [depth, D],
                "ffn2_w_gate": [depth, D, F], "ffn2_w_up": [depth, D, F], "ffn2_w_down": [depth, F, D],
                "final_norm": [D],
            }
            for k, s in shapes.items():
                din[k] = self.dram_in(k, s)
            self.din = din
            big = ["ffn1_w_gate", "ffn1_w_up", "ffn1_w_down", "w_in", "w_out",
                   "ffn2_w_gate", "ffn2_w_up", "ffn2_w_down"]
            self.wbf = {k: nc.dram_tensor("bf_" + k, shapes[k], BF16, kind="Internal").ap() for k in big}

            self.xT = self.sb("xT", [P, DC, TT], F32)
            self.hT = self.sb("hT", [P, DC, TT], BF16)
            self.wb = self.sb("wb", [P, 12 * D], BF16)
            self.ring = self.sb("ring", [P, RING, 4096], BF16)
            self.wa = self.sb("wa", [P, DC, 16], BF16)
            self.rstd = self.sb("rstd", [P, TT], F32)
            self.sq = self.sb("sq", [P, 4, SUB], BF16)
            self.aaug = self.sb("aaug", [32, TT], BF16)
            self.dec = self.sb("dec", [P, 2, NCH], F32)
            self.S32 = self.sb("S32", [P, depth, 2, 128], F32)
            self.tails = self.sb("tails", [P, depth, 4, 2], F32)
            self.NG = 3 * depth * DC + DC + depth
            self.NCW = depth * 3 * 4
            assert self.NG <= P and self.NCW <= P
            self.prow = self.sb("prow", [P, P], F32)
            self.crow = self.sb("crow", [P, P], F32)
            self.gall = self.sb("gall", [P, self.NG], F32)
            self.cwall = self.sb("cwall", [P, self.NCW], F32)
            n8 = depth * DC
            self.gn1 = self.gall[:, 0:n8].rearrange("p (l c) -> p l c", l=depth)
            self.gnm = self.gall[:, n8:2 * n8].rearrange("p (l c) -> p l c", l=depth)
            self.gn2 = self.gall[:, 2 * n8:3 * n8].rearrange("p (l c) -> p l c", l=depth)
            self.gnf = self.gall[:, 3 * n8:3 * n8 + DC]
            self.gon = self.gall[:, 3 * n8 + DC:3 * n8 + DC + depth]
            self.cw = self.cwall[:, :].rearrange("p (l k c) -> p l k c", l=depth, k=3)
            self.wa2 = self.sb("wa2", [32, depth, 256], BF16)
            self.onesD = self.sb("onesD", [P, P], BF16)
            self.onesV = self.sb("onesV", [P, P], BF16)
            self.ident = self.sb("ident", [P, P], F32)
            self.triA = self.sb("triA", [P, P], F32)
            self.triB = self.sb("triB", [P, P], F32)
            self.mask4 = self.sb("mask4", [P, 4, P], F32)
            ARENA_BYTES = 86 * 1024
            self.arena = self.sb("arena", [P, ARENA_BYTES // 2], BF16)

            def carve(layout):
                off = 0
                for name, shape, dt, parts in layout:
                    n = 1
                    for d_ in shape:
                        n *= d_
                    nb = n * (4 if dt == F32 else 2)
                    v = self.arena[0:parts, off // 2:(off + nb) // 2]
                    if dt == F32:
                        v = v.bitcast(F32)
                    if len(shape) == 2:
                        v = v.rearrange("p (a b) -> p a b", a=shape[0])
                    elif len(shape) == 3:
                        v = v.rearrange("p (a b c) -> p a b c", a=shape[0], b=shape[1])
                    setattr(self, name, v)
                    off += (nb + 63) // 64 * 64
                assert off <= ARENA_BYTES, off
            carve([("act", (12, TT), BF16, P), ("tmpf", (3, SUB), F32, P)])
            carve([("stage", (2, D), F32, P), ("ostage", (2, D), F32, P), ("yT", (DC, P), F32, P),
                   ("wa2f", (depth, 256), F32, 32)])
            carve([("qt", (4, TT), BF16, P), ("kt", (2, TT), BF16, P), ("vtok", (NCH, 512), BF16, P),
                   ("khat", (NCH, 256), BF16, P), ("sg", (4, TT), BF16, P), ("mixT", (DC, TT), BF16, P),
                   ("Lb", (2, 512), F32, P), ("eb", (2, SUB), F32, P), ("enb", (2, SUB), F32, P),
                   ("ebd", (2, SUB), F32, P), ("AT", (2, 512), BF16, P), ("sqo", (2, 512), BF16, P),
                   ("rso", (2, 512), F32, P), ("cu", (2, 2 + SUB), F32, P), ("cacc", (2, SUB), F32, P),
                   ("Sbf", (NCH, 2, 128), BF16, P)])
            self.ps = [es.enter_context(nc.psum_tensor("ps%d" % i, [P, 512], F32)) for i in range(8)]

            self.prologue()
            self.param_transposes()
            for ti in range(self.ntiles):
                phases = []
                for l in range(depth):
                    phases += [("f0", l, (self.gn1, l, False)), ("mx", l, (self.gnm, l, False)), ("f1", l, (self.gn2, l, False))]
                phases.append(("fin", None, (self.gnf, None, True)))
                if ti == 0:
                    self.load_tile(ti)
                self.norm_s(phases[0][2], 0)
                self.defer(lambda spec=phases[0][2]: self.norm_s(spec, 1))
                for i, (kind, l, spec) in enumerate(phases[:-1]):
                    nxt = phases[i + 1][2]
                    if kind == "f0":
                        self.ffn(l, 0, ti, nxt)
                    elif kind == "mx":
                        self.mixer(l, ti, nxt)
                    else:
                        self.ffn(l, 1, ti, nxt)
                if ti + 1 < self.ntiles:
                    self.store_tile(ti, hook=lambda tc, ti=ti: self.load_tile(ti + 1, [tc - 4]) if tc >= 4 else None)
                    self.load_tile(ti + 1, range(4, NCH))
                else:
                    self.store_tile(ti)
            self.tr.final_wait("act")
            self.sbuf_left = nc.sbuf_bytes_remaining
            self.tr.emit(nc, es)
        return nc

    def prologue(self):
        depth = self.depth
        din = self.din
        self.pending = []
        for l in range(depth):
            for gname in ("f0", "mx", "f1"):
                pcs = self.cast_pieces(l, gname)
                if l == 0 and gname == "f0":
                    for pc in pcs:
                        self.issue_cast(pc, None)
                else:
                    self.pending.append(pcs)
        n8 = depth * DC

        def fnp(e, sem):
            e.dma_start(out=self.prow[0:n8, :], in_=din["ffn1_norm"].rearrange("l (c p) -> (l c) p", p=P)).then_inc(sem, 16)
            e.dma_start(out=self.prow[n8:2 * n8, :], in_=din["mix_norm"].rearrange("l (c p) -> (l c) p", p=P)).then_inc(sem, 16)
            e.dma_start(out=self.prow[2 * n8:3 * n8, :], in_=din["ffn2_norm"].rearrange("l (c p) -> (l c) p", p=P)).then_inc(sem, 16)
            e.dma_start(out=self.prow[3 * n8:3 * n8 + DC, :], in_=din["final_norm"].rearrange("(c p) -> c p", p=P)).then_inc(sem, 16)
            e.dma_start(out=self.prow[3 * n8 + DC:self.NG, :], in_=din["gla_out_norm"]).then_inc(sem, 16)
            e.dma_start(out=self.crow[0:self.NCW, :], in_=din["conv_w"].rearrange("l k (c p) -> (l k c) p", p=P)).then_inc(sem, 16)
        self.tr.dma("sp", "par", 6, fnp, W=["prow"])
        self._param_transpose_pending = True
        self.POOL(lambda e: e.memset(self.wa2f[:, :, :], 0.0), W=["wa2f"])

        def fna(e, sem):
            e.dma_start(out=self.wa2f[0:16, :, :], in_=din["w_a2"].rearrange("l r n -> r l n")).then_inc(sem, 16)
            e.dma_start(out=self.wa2f[16:17, :, :], in_=din["b_a"].rearrange("(o l) n -> o l n", o=1)).then_inc(sem, 16)
        self.tr.dma("sp", "par2", 2, fna, W=["wa2f"])
        self.POOL(lambda e: e.tensor_copy(out=self.wa2[:, :, :], in_=self.wa2f[:, :, :]), R=["wa2f"], W=["wa2"])
        self.POOL(lambda e: e.memset(self.onesD[:, :], 1.0 / D), W=["onesD"])
        self.POOL(lambda e: e.memset(self.onesV[:, :], 1.0 / 128.0), W=["onesV"])
        self.POOL(lambda e: e.memset(self.aaug[:, :], 1.0), W=[("aaug", 0), ("aaug", 1)])
        self.POOL(lambda e: e.memset(self.S32[:, :, :, :], 0.0),
                  W=[("S32", l, a_, b_) for l in range(depth) for a_ in range(2) for b_ in range(2)])
        self.POOL(lambda e: e.memset(self.tails[:, :, :, :], 0.0), W=[("tails", l) for l in range(depth)])
        self.POOL(lambda e: e.memset(self.ident[:, :], 1.0), W=["ident"])
        self.POOL(lambda e: e.affine_select(out=self.ident[:, :], in_=self.ident[:, :], pattern=[[-1, P]],
                                            compare_op=ALU.is_equal, fill=0.0, base=0, channel_multiplier=1),
                  R=["ident"], W=["ident"])
        self.POOL(lambda e: e.memset(self.triA[:, :], -1.0 / 16.0), W=["triA"])
        self.POOL(lambda e: e.affine_select(out=self.triA[:, :], in_=self.triA[:, :], pattern=[[1, P]],
                                            compare_op=ALU.is_ge, fill=0.0, base=0, channel_multiplier=-1),
                  R=["triA"], W=["triA"])
        self.POOL(lambda e: e.memset(self.triB[:, :], -1.0 / 16.0), W=["triB"])
        self.POOL(lambda e: e.affine_select(out=self.triB[:, :], in_=self.triB[:, :], pattern=[[-1, P]],
                                            compare_op=ALU.is_ge, fill=0.0, base=-1, channel_multiplier=1),
                  R=["triB"], W=["triB"])
        self.POOL(lambda e: e.memset(self.mask4[:, :, :], 1.0), W=["mask4"])
        self.POOL(lambda e: e.affine_select(out=self.mask4[:, :, :], in_=self.mask4[:, :, :], pattern=[[0, 4], [1, P]],
                                            compare_op=ALU.is_ge, fill=0.0, base=0, channel_multiplier=-1),
                  R=["mask4"], W=["mask4"])

    def cast_pieces(self, l, gname):
        din, wbf = self.din, self.wbf
        out = []
        if gname in ("f0", "f1"):
            pre = "ffn1" if gname == "f0" else "ffn2"
            for b in range(4):
                c0, c1 = b * 704, (b + 1) * 704
                out.append((("wbf", l, gname, "gu%d" % b),
                            [(wbf[pre + "_w_gate"][l][:, c0:c1], din[pre + "_w_gate"][l][:, c0:c1]),
                             (wbf[pre + "_w_up"][l][:, c0:c1], din[pre + "_w_up"][l][:, c0:c1])]))
                if b == 2:
                    out.append((("wbf", l, gname, "d0"),
                                [(wbf[pre + "_w_down"][l][0:1536, :], din[pre + "_w_down"][l][0:1536, :])]))
            out.append((("wbf", l, gname, "d1"),
                        [(wbf[pre + "_w_down"][l][1536:F, :], din[pre + "_w_down"][l][1536:F, :])]))
        else:
            for nm, c0, c1 in (("a", 1536, 1552), ("v", 512, 1024), ("qk", 0, 512), ("g", 1024, 1536),
                               ("u", 1552, 2064), ("b", 2064, 2576), ("c", 2576, 3088)):
                out.append((("wbf", l, gname, nm), [(wbf["w_in"][l][:, c0:c1], din["w_in"][l][:, c0:c1])]))
            out.append((("wbf", l, gname, "o"), [(wbf["w_out"][l], din["w_out"][l])]))
        return out

    def wkey(self, key):
        return key if (key[1], key[2]) == (0, "f0") else key[:3]

    def issue_cast(self, piece, tickkey):
        key, pairs = piece
        if (key[1], key[2]) == (0, "f0"):
            sem = "c0_%d" % self.cast_i
            self.cast_i += 1
        else:
            sem = "cg%d%s" % (key[1], key[2])
        key = self.wkey(key)

        def fn(e, s, pairs=pairs):
            for dst, src_ in pairs:
                e.dma_start(out=dst, in_=src_).then_inc(s, 16)
        self.tr.dma("pool", sem, len(pairs), fn, R=([tickkey] if tickkey is not None else []), W=[key])

    def begin_phase(self, nticks):
        self.cur_pieces = []
        if self.pending:
            if self.phase_i == 0:
                self.cur_pieces += self.pending.pop(0)
            if self.pending:
                self.cur_pieces += self.pending.pop(0)
        self.per_tick = -(-len(self.cur_pieces) // nticks) if self.cur_pieces else 0
        self.phase_i += 1

    def tick(self, flush=False):
        if not self.cur_pieces:
            return
        key = ("tick", self.tick_i)
        self.tick_i += 1
        self.tr.lastw[key] = ("pe", self.tr.nops["pe"] - 1)
        n = len(self.cur_pieces) if flush else self.per_tick
        for _ in range(min(n, len(self.cur_pieces))):
            self.issue_cast(self.cur_pieces.pop(0), key)

    def param_transposes(self):
        for rows, n, dst in ((self.prow, self.NG, self.gall), (self.crow, self.NCW, self.cwall)):
            bk, ps = self.bank()
            self.mm(ps[:, 0:n], rows[0:n, :], self.ident[0:n, 0:n], True, True, R=["prow", "ident"], W=[bk])
            self.ACT(lambda e, ps=ps, n=n, dst=dst: e.activation(out=dst[:, 0:n], in_=ps[:, 0:n], func=AF.Copy),
                     R=[bk], W=["par"])

    def load_tile(self, ti, tcs=None):
        tok0 = ti * TT
        for tc in (range(NCH) if tcs is None else tcs):
            slot = tc % 2
            src = self.x[tok0 + tc * P: tok0 + (tc + 1) * P, :]
            self.tr.dma("sp", "xin%d" % slot, 1,
                        lambda e, sem, slot=slot, src=src: e.dma_start(out=self.stage[:, slot, :], in_=src).then_inc(sem, 16),
                        W=[("stage", slot)])
            for g in range(2):
                bk, ps = self.bank()
                for j in range(4):
                    dc = 4 * g + j
                    self.PE(lambda e, ps=ps, j=j, dc=dc, slot=slot: e.transpose(
                        ps[:, j * P:(j + 1) * P], self.stage[:, slot, dc * P:(dc + 1) * P], self.ident[:, :]),
                        R=[("stage", slot), "ident"], W=[bk])
                self.ACT(lambda e, ps=ps, g=g, tc=tc: e.activation(
                    out=self.xT[:, 4 * g:4 * g + 4, tc * P:(tc + 1) * P],
                    in_=ps[:, :].rearrange("p (a b) -> p a b", a=4), func=AF.Copy),
                    R=[bk], W=[("xT", dc, tc // 4) for dc in range(4 * g, 4 * g + 4)])

    def store_tile(self, ti, hook=None):
        tok0 = ti * TT
        self.flush_deferred()
        self.phase("io")
        for tc in range(NCH):
            s = tc // 4
            slot = tc % 2
            for dc in range(DC):
                eng = self.DVE
                eng(lambda e, dc=dc, tc=tc: e.scalar_tensor_tensor(
                    out=self.yT[:, dc, :], in0=self.xT[:, dc, tc * P:(tc + 1) * P], scalar=self.gnf[:, dc:dc + 1],
                    in1=self.rstd[:, tc * P:(tc + 1) * P], op0=ALU.mult, op1=ALU.mult),
                    R=[("xT", dc, s), ("rstd", s), "par"], W=[("yT", dc)])
            for g in range(2):
                bk, ps = self.bank()
                for j in range(4):
                    dc = 4 * g + j
                    self.PE(lambda e, ps=ps, j=j, dc=dc: e.transpose(
                        ps[:, j * P:(j + 1) * P], self.yT[:, dc, :], self.ident[:, :]),
                        R=[("yT", dc), "ident"], W=[bk])
                self.ACT(lambda e, ps=ps, g=g, slot=slot: e.activation(
                    out=self.ostage[:, slot, g * 512:(g + 1) * 512], in_=ps[:, :], func=AF.Copy),
                    R=[bk], W=[("ostage", slot)])
            dst = self.y[tok0 + tc * P: tok0 + (tc + 1) * P, :]
            self.tr.dma("act", "out%d" % slot, 1,
                        lambda e, sem, slot=slot, dst=dst: e.dma_start(out=dst, in_=self.ostage[:, slot, :]).then_inc(sem, 16),
                        R=[("ostage", slot)])
            if hook is not None:
                hook(tc)

    def norm_s(self, spec, s):
        gain, l, final = spec
        cs = slice(s * SUB, (s + 1) * SUB)
        bk, ps = self.bank()
        for dc in range(DC):
            q = self.uid % 4
            self.uid += 1
            self.ACT(lambda e, dc=dc, q=q, cs=cs: e.activation(out=self.sq[:, q, :], in_=self.xT[:, dc, cs], func=AF.Square),
                     R=[("xT", dc, s)], W=[("sq", q)])
            self.mm(ps[:, :], self.onesD[:, :], self.sq[:, q, :], dc == 0, dc == DC - 1,
                    R=[("sq", q), "onesD"], W=[bk])
        self.ACT(lambda e, ps=ps, cs=cs: e.activation(out=self.rstd[:, cs], in_=ps[:, :], func=AF.Ln, bias=EPS),
                 R=[bk], W=[("rstd", s)])
        self.ACT(lambda e, cs=cs: e.activation(out=self.rstd[:, cs], in_=self.rstd[:, cs], func=AF.Exp, scale=-0.5),
                 R=[("rstd", s)], W=[("rstd", s)])
        if final:
            return
        for dc in range(DC):
            g_ap = gain[:, l, dc:dc + 1]
            self.DVE(lambda e, dc=dc, cs=cs, g_ap=g_ap: e.scalar_tensor_tensor(
                out=self.hT[:, dc, cs], in0=self.xT[:, dc, cs], scalar=g_ap, in1=self.rstd[:, cs],
                op0=ALU.mult, op1=ALU.mult),
                R=[("xT", dc, s), ("rstd", s), "par"], W=[("hT", dc, s)])

    def defer(self, fn):
        self.deferred.append(fn)

    def flush_deferred(self):
        d, self.deferred = self.deferred, []
        for fn in d:
            fn()

    def residual_tail(self, emit_group, nxt):
        for s in range(NS):
            for dc in range(DC):
                emit_group(s, dc)
                if s == 1 and dc == 1:
                    self.norm_s(nxt, 0)
        self.defer(lambda: self.norm_s(nxt, 1))

    def slab(self, parts, R):
        k = self.slab_i
        self.slab_i += 1
        slot = k % RING
        sl = self.ring[:, slot, :]

        def fn(e, sem, parts=parts, sl=sl):
            for dstf, src in parts:
                e.dma_start(out=dstf(sl), in_=src).then_inc(sem, 16)
        self.tr.dma("sp", "ring%d" % slot, len(parts), fn, R=R, W=[("ring", slot)])
        return ("ring", slot), sl

    def load_wb(self, view_fn, src, R):
        def fn(e, sem):
            e.dma_start(out=view_fn(self.wb[:, :]), in_=src).then_inc(sem, 16)
        self.tr.dma("act", "wb", 1, fn, R=R, W=["wb"])

    def ffn(self, l, which, ti, nxt):
        pre = "ffn1" if which == 0 else "ffn2"
        gname = "f0" if which == 0 else "f1"
        self.begin_phase(13)
        wg = self.wbf[pre + "_w_gate"][l]
        wu = self.wbf[pre + "_w_up"][l]
        wd = self.wbf[pre + "_w_down"][l]
        gain = self.gn1 if which == 0 else self.gn2
        self.phase("ffn")
        slab0 = 0
        for half, nsl in enumerate(HALF_SLABS):
            nfc = nsl * 2
            fc0 = slab0 * 2
            self.load_wb(lambda w, nfc=nfc: w[:, 0:nfc * D].rearrange("p (j d) -> p j d", j=nfc),
                         wd[fc0 * P:(fc0 + nfc) * P, :].rearrange("(j p) d -> p j d", p=P), R=[self.wkey(("wbf", l, gname, "d%d" % half))])
            def load_gu(si, slab0=slab0):
                col0 = (slab0 + si) * FS
                rk, sl = self.slab([
                    (lambda s_: s_[:, 0:2048].rearrange("p (k c) -> p k c", k=DC), wg[:, col0:col0 + FS].rearrange("(k p) c -> p k c", p=P)),
                    (lambda s_: s_[:, 2048:4096].rearrange("p (k c) -> p k c", k=DC), wu[:, col0:col0 + FS].rearrange("(k p) c -> p k c", p=P)),
                ], R=[self.wkey(("wbf", l, gname, "gu%d" % b)) for b in sorted({col0 // 704, (col0 + FS - 1) // 704})])
                gv = sl[:, 0:2048].rearrange("p (k c) -> p k c", k=DC)
                uv = sl[:, 2048:4096].rearrange("p (k c) -> p k c", k=DC)
                return rk, gv, uv

            def gu_item(slabinfo, si, c, s):
                rk, gv, uv = slabinfo
                fcl = si * 2 + c
                cs = slice(s * SUB, (s + 1) * SUB)
                bg, pg = self.bank()
                bu, pu = self.bank()
                for kc in range(DC):
                    self.mm(pg[:, :], gv[:, kc, c * P:(c + 1) * P], self.hT[:, kc, cs], kc == 0, kc == DC - 1,
                            R=[rk, ("hT", kc, s)], W=[bg])
                for kc in range(DC):
                    self.mm(pu[:, :], uv[:, kc, c * P:(c + 1) * P], self.hT[:, kc, cs], kc == 0, kc == DC - 1,
                            R=[rk, ("hT", kc, s)], W=[bu])
                q = self.uid % 3
                self.uid += 1
                self.ACT(lambda e, pg=pg, q=q: e.activation(out=self.tmpf[:, q, :], in_=pg[:, :], func=AF.Silu),
                         R=[bg], W=[("tmpf", q)])
                self.DVE(lambda e, pu=pu, q=q, fcl=fcl, cs=cs: e.tensor_tensor(
                    out=self.act[:, fcl, cs], in0=pu[:, :], in1=self.tmpf[:, q, :], op=ALU.mult),
                    R=[bu, ("tmpf", q)], W=[("act", fcl, s)])

            si0 = 0
            if half == 0:
                sA, sB = load_gu(0), load_gu(1)
                gu_item(sA, 0, 0, 0)
                gu_item(sA, 0, 1, 0)
                self.flush_deferred()
                gu_item(sB, 1, 0, 0)
                gu_item(sB, 1, 1, 0)
                self.tick()
                gu_item(sA, 0, 0, 1)
                gu_item(sA, 0, 1, 1)
                gu_item(sB, 1, 0, 1)
                gu_item(sB, 1, 1, 1)
                self.tick()
                si0 = 2
            for si in range(si0, nsl):
                info = load_gu(si)
                for c in range(2):
                    for s in range(NS):
                        gu_item(info, si, c, s)
                self.tick()
            wv = self.wb[:, 0:nfc * D].rearrange("p (j d) -> p j d", j=nfc)

            def down_group(s, dc, wv=wv, nfc=nfc):
                cs = slice(s * SUB, (s + 1) * SUB)
                bk, ps = self.bank()
                for j in range(nfc):
                    self.mm(ps[:, :], wv[:, j, dc * P:(dc + 1) * P], self.act[:, j, cs], j == 0, j == nfc - 1,
                            R=["wb", ("act", j, s)], W=[bk])
                self.DVE(lambda e, ps=ps, dc=dc, cs=cs: e.scalar_tensor_tensor(
                    out=self.xT[:, dc, cs], in0=ps[:, :], scalar=0.5, in1=self.xT[:, dc, cs],
                    op0=ALU.mult, op1=ALU.add),
                    R=[bk, ("xT", dc, s)], W=[("xT", dc, s)])
            if half == 0:
                for s in range(NS):
                    for dc in range(DC):
                        down_group(s, dc)
            else:
                self.residual_tail(down_group, nxt)
            self.tick(flush=(half == 1))
            slab0 += nsl

    def mixer(self, l, ti, nxt):
        def grp(*names):
            return [self.wkey(("wbf", l, "mx", nm)) for nm in names]
        self.begin_phase(16)
        win = self.wbf["w_in"][l]
        wout = self.wbf["w_out"][l]
        self.phase("mix")
        self.POOL(lambda e: e.memset(self.qt[:, :, :], 0.0), W=[("qt", kc2, s) for kc2 in range(2) for s in range(NS)])

        def wcols(c0, n):
            return win[:, c0:c0 + n].rearrange("(k p) c -> p k c", p=P)

        def fa(e, sem):
            e.dma_start(out=self.wa[:, :, :], in_=wcols(1536, 16)).then_inc(sem, 16)
        self.tr.dma("sp", "wa", 1, fa, R=grp("a"), W=["wa"])
        def alr(s):
            cs = slice(s * SUB, (s + 1) * SUB)
            bk, ps = self.bank()
            for kc in range(DC):
                self.mm(ps[0:16, :], self.wa[:, kc, :], self.hT[:, kc, cs], kc == 0, kc == DC - 1,
                        R=["wa", ("hT", kc, s)], W=[bk])
            self.ACT(lambda e, ps=ps, cs=cs: e.activation(out=self.aaug[0:16, cs], in_=ps[0:16, :], func=AF.Copy),
                     R=[bk], W=[("aaug", s)])
        alr(0)
        rv, slv = self.slab([(lambda s_: s_[:, :].rearrange("p (k c) -> p k c", k=DC), wcols(512, 512))], R=grp("v"))
        vv = slv[:, :].rearrange("p (k c) -> p k c", k=DC)
        rqk, slqk = self.slab([(lambda s_: s_[:, :].rearrange("p (k c) -> p k c", k=DC), wcols(0, 512))], R=grp("qk"))
        qkv = slqk[:, :].rearrange("p (k c) -> p k c", k=DC)

        def vproj(n):
            bk, ps = self.bank()
            for kc in range(DC):
                self.mm(ps[:, :], self.hT[:, kc, n * P:(n + 1) * P], vv[:, kc, :], kc == 0, kc == DC - 1,
                        R=[rv, ("hT", kc, n // 4)], W=[bk])
            self.ACT(lambda e, ps=ps, n=n: e.activation(out=self.vtok[:, n, :], in_=ps[:, :], func=AF.Copy),
                     R=[bk], W=[("vtok", n)])

        bt_banks = {}
        for pr in range(NCH // 2):
            s = pr // 2
            if pr == 1:
                self.flush_deferred()
            if pr == 2:
                alr(1)
            if pr % 2 == 0:
                for kc2 in range(2):
                    bt_banks[(s, kc2)] = self.bank(hold=True)
            bkz, psz = self.bank()
            for j in range(2):
                n = 2 * pr + j
                self.mm(psz[:, j * 256:(j + 1) * 256], self.aaug[:, n * P:(n + 1) * P], self.wa2[:, l, :], True, True,
                        R=[("aaug", s), "wa2"], W=[bkz])
            lq = pr % 2
            self.ACT(lambda e, psz=psz, lq=lq: e.activation(out=self.Lb[:, lq, :], in_=psz[:, :], func=AF.Exp, scale=-1.0),
                     R=[bkz], W=[("Lb", lq)])
            self.ACT(lambda e, lq=lq: e.activation(out=self.Lb[:, lq, :], in_=self.Lb[:, lq, :], func=AF.Ln, bias=1.0),
                     R=[("Lb", lq)], W=[("Lb", lq)])
            vproj(2 * pr)
            vproj(2 * pr + 1)
            bkd, psd = self.bank()
            for j in range(2):
                n = 2 * pr + j
                nl = n % 4
                Lv = self.Lb[:, lq, j * 256:(j + 1) * 256]
                for kc2 in range(2):
                    bkt, pst = bt_banks[(s, kc2)]
                    self.mm(pst[:, nl * P:(nl + 1) * P], Lv[:, kc2 * P:(kc2 + 1) * P], self.triA[:, :], True, True,
                            R=[("Lb", lq), "triA"], W=[bkt])
                self.mm(psd[:, j * 256:(j + 1) * 256], self.triB[:, :], Lv, True, True,
                        R=[("Lb", lq), "triB"], W=[bkd])
            bkk, psk = self.bank()
            for j in range(2):
                n = 2 * pr + j
                for kc in range(DC):
                    self.mm(psk[:, j * 256:(j + 1) * 256], self.hT[:, kc, n * P:(n + 1) * P], qkv[:, kc, 256:512],
                            kc == 0, kc == DC - 1, R=[rqk, ("hT", kc, s)], W=[bkk])
            eq = pr % 2
            self.ACT(lambda e, psd=psd, eq=eq: e.activation(out=self.ebd[:, eq, :], in_=psd[:, :], func=AF.Exp),
                     R=[bkd], W=[("ebd", eq)])
            self.DVE(lambda e, psk=psk, eq=eq, pr=pr: e.tensor_tensor(
                out=self.khat[:, 2 * pr:2 * pr + 2, :], in0=psk[:, :].rearrange("p (a b) -> p a b", a=2),
                in1=self.ebd[:, eq, :].rearrange("p (a b) -> p a b", a=2), op=ALU.mult),
                R=[bkk, ("ebd", eq)], W=[("khat", 2 * pr), ("khat", 2 * pr + 1)])
            if pr % 2 == 1:
                cs = slice(s * SUB, (s + 1) * SUB)
                for kc2 in range(2):
                    bkt, pst = bt_banks[(s, kc2)]
                    self.ACT(lambda e, pst=pst, kc2=kc2: e.activation(out=self.eb[:, kc2, :], in_=pst[:, :], func=AF.Exp),
                             R=[bkt], W=[("eb", kc2)])
                    self.ACT(lambda e, pst=pst, kc2=kc2: e.activation(out=self.enb[:, kc2, :], in_=pst[:, :], func=AF.Exp, scale=-1.0),
                             R=[bkt], W=[("enb", kc2)])
                    self.DVE(lambda e, kc2=kc2, s=s: e.tensor_copy(out=self.dec[:, kc2, 4 * s:4 * s + 4], in_=self.eb[:, kc2, 127:512:128]),
                             R=[("eb", kc2)], W=[("dec", kc2, s)])
                    bq, pq = self.bank()
                    for kc in range(DC):
                        self.mm(pq[:, :], qkv[:, kc, kc2 * P:(kc2 + 1) * P], self.hT[:, kc, cs], kc == 0, kc == DC - 1,
                                R=[rqk, ("hT", kc, s)], W=[bq])
                    for hl in range(2):
                        pr_ = slice(hl * 64, (hl + 1) * 64)
                        self.DVE(lambda e, pq=pq, kc2=kc2, cs=cs, pr_=pr_, hl=hl: e.scalar_tensor_tensor(
                            out=self.qt[pr_, 2 * kc2 + hl, cs], in0=pq[pr_, :], scalar=0.125, in1=self.eb[pr_, kc2, :],
                            op0=ALU.mult, op1=ALU.mult),
                            R=[bq, ("eb", kc2)], W=[("qt", kc2, s)])
                    bk_, pk = self.bank()
                    for kc in range(DC):
                        self.mm(pk[:, :], qkv[:, kc, 256 + kc2 * P:256 + (kc2 + 1) * P], self.hT[:, kc, cs], kc == 0, kc == DC - 1,
                                R=[rqk, ("hT", kc, s)], W=[bk_])
                    self.DVE(lambda e, pk=pk, kc2=kc2, cs=cs: e.tensor_tensor(
                        out=self.kt[:, kc2, cs], in0=pk[:, :], in1=self.enb[:, kc2, :], op=ALU.mult),
                        R=[bk_, ("enb", kc2)], W=[("kt", kc2, s)])
                    self.held.discard(bkt[1])
            self.tick()
        rg, slg = self.slab([(lambda s_: s_[:, :].rearrange("p (k c) -> p k c", k=DC), wcols(1024, 512))], R=grp("g"))
        gvw = slg[:, :].rearrange("p (k c) -> p k c", k=DC)
        for n in range(NCH):
            s = n // 4
            bku, psu = self.bank()
            for kc2 in range(2):
                self.mm(psu[:, kc2 * 256:(kc2 + 1) * 256], self.khat[:, n, kc2 * P:(kc2 + 1) * P],
                        self.vtok[:, n, kc2 * 256:(kc2 + 1) * 256], True, True,
                        R=[("khat", n), ("vtok", n)], W=[bku])
            self.POOL(lambda e, n=n: e.tensor_copy(out=self.Sbf[:, n, :, :], in_=self.S32[:, l, :, :]),
                      R=[("S32", l, a_, b_) for a_ in range(2) for b_ in range(2)], W=[("Sbf", n)])
            for kc2 in range(2):
                for hl in range(2):
                    pr_ = slice(hl * 64, (hl + 1) * 64)
                    c0 = kc2 * 256 + hl * 128
                    self.DVE(lambda e, psu=psu, kc2=kc2, pr_=pr_, c0=c0, n=n: e.scalar_tensor_tensor(
                        out=self.S32[pr_, l, kc2, :], in0=self.S32[pr_, l, kc2, :], scalar=self.dec[pr_, kc2, n:n + 1],
                        in1=psu[pr_, c0:c0 + 128], op0=ALU.mult, op1=ALU.add),
                        R=[bku, ("S32", l, kc2, hl), ("dec", kc2, s)], W=[("S32", l, kc2, hl)])
            sg_s, sg_c = n // 4, n % 4
            cs = slice(sg_s * SUB, (sg_s + 1) * SUB)
            bk, ps = self.bank()
            for kc in range(DC):
                self.mm(ps[:, :], gvw[:, kc, sg_c * P:(sg_c + 1) * P], self.hT[:, kc, cs], kc == 0, kc == DC - 1,
                        R=[rg, ("hT", kc, sg_s)], W=[bk])
            self.ACT(lambda e, ps=ps, sg_c=sg_c, cs=cs: e.activation(out=self.sg[:, sg_c, cs], in_=ps[:, :], func=AF.Silu),
                     R=[bk], W=[("sg", sg_c, sg_s)])
        self.tick()
        self.load_wb(lambda w: w[:, 0:DC * D].rearrange("p (k d) -> p k d", k=DC),
                     wout.rearrange("(k p) d -> p k d", p=P), R=grp("o"))
        wov = self.wb[:, 0:DC * D].rearrange("p (k d) -> p k d", k=DC)

        def conv_item(c, s):
            rc, slc = self.slab([
                (lambda s_, j=j: s_[:, j * 1024:(j + 1) * 1024].rearrange("p (k c) -> p k c", k=DC),
                 wcols(1552 + 512 * j + c * P, P)) for j in range(3)], R=grp("u", "b", "c"))
            cs = slice(s * SUB, (s + 1) * SUB)
            pss = []
            for j in range(3):
                wvj = slc[:, j * 1024:(j + 1) * 1024].rearrange("p (k c) -> p k c", k=DC)
                bk, ps = self.bank()
                for kc in range(DC):
                    self.mm(ps[:, :], wvj[:, kc, :], self.hT[:, kc, cs], kc == 0, kc == DC - 1,
                            R=[rc, ("hT", kc, s)], W=[bk])
                pss.append((bk, ps))
            (bu, pu), (bb, pb), (bc, pc) = pss
            q = self.uid % 2
            self.uid += 1
            self.ACT(lambda e, pc=pc, q=q: e.activation(out=self.cacc[:, q, :], in_=pc[:, :], func=AF.Copy),
                     R=[bc], W=[("cacc", q)])
            self.ACT(lambda e, pb=pb, q=q: e.activation(out=self.ebd[:, q, :], in_=pb[:, :], func=AF.Copy),
                     R=[bb], W=[("ebd", q)])
            self.POOL(lambda e, q=q, c=c: e.tensor_copy(out=self.cu[:, q, 0:2], in_=self.tails[:, l, c, :]),
                      R=[("tails", l, c)], W=[("cu", q)])
            self.DVE(lambda e, pu=pu, q=q: e.tensor_tensor(out=self.cu[:, q, 2:2 + SUB], in0=pu[:, :], in1=self.cacc[:, q, :], op=ALU.mult),
                     R=[bu, ("cacc", q), ("cu", q)], W=[("cu", q)])
            self.POOL(lambda e, q=q, c=c: e.tensor_copy(out=self.tails[:, l, c, :], in_=self.cu[:, q, SUB:SUB + 2]),
                      R=[("cu", q)], W=[("tails", l, c)])
            self.ACT(lambda e, q=q, c=c: e.activation(out=self.cacc[:, q, :], in_=self.cu[:, q, 2:2 + SUB], func=AF.Copy,
                                                      scale=self.cw[:, l, 2, c:c + 1]),
                     R=[("cu", q), "par"], W=[("cacc", q)])
            self.DVE(lambda e, q=q, c=c: e.scalar_tensor_tensor(out=self.cacc[:, q, :], in0=self.cu[:, q, 1:1 + SUB],
                                                                scalar=self.cw[:, l, 1, c:c + 1], in1=self.cacc[:, q, :],
                                                                op0=ALU.mult, op1=ALU.add),
                     R=[("cu", q), ("cacc", q), "par"], W=[("cacc", q)])
            self.DVE(lambda e, q=q, c=c: e.scalar_tensor_tensor(out=self.cacc[:, q, :], in0=self.cu[:, q, 0:SUB],
                                                                scalar=self.cw[:, l, 0, c:c + 1], in1=self.cacc[:, q, :],
                                                                op0=ALU.mult, op1=ALU.add),
                     R=[("cu", q), ("cacc", q), "par"], W=[("cacc", q)])
            self.DVE(lambda e, q=q, c=c, cs=cs: e.tensor_tensor(out=self.mixT[:, 4 + c, cs], in0=self.ebd[:, q, :], in1=self.cacc[:, q, :], op=ALU.mult),
                     R=[("ebd", q), ("cacc", q)], W=[("mixT", 4 + c, s)])

        def gla_scores(n):
            s = n // 4
            cn = slice(n * P, (n + 1) * P)
            bsc, psc = self.bank()
            for h in range(4):
                kc2 = h // 2
                self.mm(psc[:, h * P:(h + 1) * P], self.kt[:, kc2, cn], self.qt[:, h, cn], True, True,
                        R=[("kt", kc2, s), ("qt", kc2, s)], W=[bsc])
            q = n % 2
            self.DVE(lambda e, psc=psc, q=q: e.tensor_tensor(
                out=self.AT[:, q, :], in0=psc[:, :], in1=self.mask4[:, :, :].rearrange("p a b -> p (a b)"), op=ALU.mult),
                R=[bsc, "mask4"], W=[("AT", q)])

        def gla_out(n):
            s = n // 4
            cn = slice(n * P, (n + 1) * P)
            q = n % 2
            bo, po = self.bank()
            for h in range(4):
                kc2 = h // 2
                self.mm(po[:, h * P:(h + 1) * P], self.vtok[:, n, h * P:(h + 1) * P], self.AT[:, q, h * P:(h + 1) * P], True, False,
                        R=[("vtok", n), ("AT", q)], W=[bo])
                self.mm(po[:, h * P:(h + 1) * P], self.Sbf[:, n, kc2, :], self.qt[:, h, cn], False, True,
                        R=[("Sbf", n), ("qt", kc2, s)], W=[bo])
            self.ACT(lambda e, po=po, q=q: e.activation(out=self.sqo[:, q, :], in_=po[:, :], func=AF.Square),
                     R=[bo], W=[("sqo", q)])
            self.ACT(lambda e, po=po, q=q: e.activation(out=self.Lb[:, q, :], in_=po[:, :], func=AF.Copy),
                     R=[bo], W=[("Lb", q)])

        def gla_norm(n):
            s = n // 4
            cn = slice(n * P, (n + 1) * P)
            q = n % 2
            bst, pst = self.bank()
            self.mm(pst[:, :], self.onesV[:, :], self.sqo[:, q, :], True, True, R=[("sqo", q), "onesV"], W=[bst])
            self.ACT(lambda e, pst=pst, q=q: e.activation(out=self.rso[:, q, :], in_=pst[:, :], func=AF.Ln, bias=EPS),
                     R=[bst], W=[("rso", q)])
            self.ACT(lambda e, q=q: e.activation(out=self.rso[:, q, :], in_=self.rso[:, q, :], func=AF.Exp, scale=-0.5),
                     R=[("rso", q)], W=[("rso", q)])
            self.DVE(lambda e, q=q: e.scalar_tensor_tensor(out=self.rso[:, q, :], in0=self.Lb[:, q, :], scalar=self.gon[:, l:l + 1],
                                                           in1=self.rso[:, q, :], op0=ALU.mult, op1=ALU.mult),
                     R=[("Lb", q), ("rso", q), "par"], W=[("rso", q)])
            self.POOL(lambda e, q=q, cn=cn: e.tensor_tensor(
                out=self.mixT[:, 0:4, cn], in0=self.rso[:, q, :].rearrange("p (a b) -> p a b", a=4), in1=self.sg[:, :, cn], op=ALU.mult),
                R=[("rso", q)] + [("sg", c, s) for c in range(4)], W=[("mixT", c, s) for c in range(4)])

        def wout_group(s, dc):
            cs = slice(s * SUB, (s + 1) * SUB)
            bk, ps = self.bank()
            for kc in range(DC):
                self.mm(ps[:, :], wov[:, kc, dc * P:(dc + 1) * P], self.mixT[:, kc, cs], kc == 0, kc == DC - 1,
                        R=["wb", ("mixT", kc, s)], W=[bk])
            self.DVE(lambda e, ps=ps, dc=dc, cs=cs: e.tensor_tensor(out=self.xT[:, dc, cs], in0=ps[:, :], in1=self.xT[:, dc, cs], op=ALU.add),
                     R=[bk, ("xT", dc, s)], W=[("xT", dc, s)])

        gla_scores(0)
        for i in range(NCH + 1):
            if i + 1 < NCH:
                gla_scores(i + 1)
            if i < NCH:
                gla_out(i)
            if i >= 1:
                conv_item((i - 1) % 4, (i - 1) // 4)
                gla_norm(i - 1)
            for dc in {5: (0,), 6: (1,), 7: (2, 3), 8: (4, 5, 6, 7)}.get(i, ()):
                wout_group(0, dc)
            self.tick()
        for dc in range(DC):
            wout_group(1, dc)
            if dc == 1:
                self.norm_s(nxt, 0)
        self.defer(lambda: self.norm_s(nxt, 1))
        self.tick(flush=True)


_W_NAMES = ["ffn1_norm", "ffn1_w_gate", "ffn1_w_up", "ffn1_w_down", "mix_norm", "w_in", "w_a2", "b_a",
            "gla_out_norm", "conv_w", "w_out", "ffn2_norm", "ffn2_w_gate", "ffn2_w_up", "ffn2_w_down", "final_norm"]


def run(inputs, trace=False, stages=("f0", "mx", "f1")):
    x = np.ascontiguousarray(inputs["x"], dtype=np.float32)
    B, T, _ = x.shape
    depth = inputs["w_in"].shape[0]
    nc = Builder(T, depth, stages).build()
    w = {k: np.ascontiguousarray(inputs[k], dtype=np.float32) for k in _W_NAMES}
    in_maps = [dict(w, x=x[b]) for b in range(B)]
    res = run_bass_kernel_spmd(nc, in_maps, core_ids=list(range(B)), trace=trace)
    out = np.stack([res.results[b]["y"] for b in range(B)], axis=0)
    return out, res


def kernel(**inputs):
    out, _ = run(inputs)
    return out
```

```python
import numpy as np
from contextlib import ExitStack
import concourse.bass as bass
import concourse.mybir as mybir
from concourse.bass_utils import run_bass_kernel_spmd

F32 = mybir.dt.float32
BF16 = mybir.dt.bfloat16
AF = mybir.ActivationFunctionType
ALU = mybir.AluOpType

P = 128
D = 1024
DC = D // P
F = 2816
FS = 256
NFS = F // FS
HALF_SLABS = (6, 5)
PW = 3088
TT = 1024
SUB = 512
NS = TT // SUB
NCH = TT // P
EPS = 1e-6
RING = 3
COMPUTE = ("pe", "act", "dve", "pool")
ARENA_NAMES = {"act", "tmpf", "stage", "ostage", "yT", "wa2f", "qt", "kt", "vtok", "khat", "sg", "mixT",
               "Lb", "eb", "enb", "ebd", "AT", "sqo", "rso", "cu", "cacc", "Sbf"}


def is_arena(key):
    return (key[0] if isinstance(key, tuple) else key) in ARENA_NAMES


class Tracker:
    def __init__(self):
        self.streams = {k: [] for k in ("pe", "act", "dve", "pool", "sp")}
        self.nops = {k: 0 for k in COMPUTE}
        self.dma_cnt = {}
        self.lastw = {}
        self.readers = {}
        self.signal = {k: set() for k in COMPUTE}
        self.floor = {}

    def arena_switch(self):
        for table in (self.lastw, self.readers):
            for key in [k for k in table if is_arena(k)]:
                v = table.pop(key)
                items = [v] if table is self.lastw else list(v.items())
                for s, i in items:
                    if self.floor.get(s, -1) < i:
                        self.floor[s] = i

    def _deps(self, me, R, W):
        deps = {}

        def add(src, idx):
            if (src, idx) == me:
                return
            if src == "pe" and me[0] == "pe":
                return
            if deps.get(src, -1) < idx:
                deps[src] = idx

        if self.floor and any(is_arena(r) for r in list(R) + list(W)):
            for s, i in self.floor.items():
                add(s, i)
        for r in R:
            w = self.lastw.get(r)
            if w is not None:
                add(*w)
        for r in W:
            w = self.lastw.get(r)
            if w is not None:
                add(*w)
            rd = self.readers.get(r)
            if rd:
                for s, i in rd.items():
                    add(s, i)
        for r in R:
            rd = self.readers.setdefault(r, {})
            if rd.get(me[0], -1) < me[1]:
                rd[me[0]] = me[1]
        for r in W:
            self.lastw[r] = me
            self.readers[r] = {}
        for s, i in deps.items():
            if s in self.signal:
                self.signal[s].add(i)
        return deps

    def op(self, eng, fn, R=(), W=()):
        idx = self.nops[eng]
        self.nops[eng] += 1
        deps = self._deps((eng, idx), R, W)
        self.streams[eng].append(("op", idx, fn, deps))

    def dma(self, queue, key, n, fn, R=(), W=()):
        cnt = self.dma_cnt.get(key, 0) + n
        self.dma_cnt[key] = cnt
        deps = self._deps(("dma:" + key, cnt), R, W)
        self.streams[queue].append(("dma", key, fn, deps))

    def final_wait(self, stream):
        deps = {"dma:" + k: c for k, c in self.dma_cnt.items() if k.startswith("out")}
        self.streams[stream].append(("wait", None, None, deps))

    def emit(self, nc, es):
        sems = {}
        for e in COMPUTE:
            sems[e] = es.enter_context(nc.semaphore("s_" + e))
        for k in self.dma_cnt:
            sems["dma:" + k] = es.enter_context(nc.semaphore("d_" + k))
        rank = {}
        for e in COMPUTE:
            rank[e] = {idx: r + 1 for r, idx in enumerate(sorted(self.signal[e]))}

        def run(stream, eng):
            waited = {}
            for kind, a, fn, deps in self.streams[stream]:
                for s, i in deps.items():
                    val = rank[s][i] if s in rank else 16 * i
                    if waited.get(s, 0) >= val:
                        continue
                    waited[s] = val
                    eng.wait_ge(sems[s], val)
                if kind == "op":
                    ins = fn(eng)
                    if a in rank[stream]:
                        ins.then_inc(sems[stream], 1)
                elif kind == "dma":
                    fn(eng, sems["dma:" + a])

        block = es.enter_context(nc.Block())

        @block.tensor
        def _(e):
            run("pe", e)

        @block.scalar
        def _(e):
            run("act", e)

        @block.vector
        def _(e):
            run("dve", e)

        @block.gpsimd
        def _(e):
            run("pool", e)

        @block.sync
        def _(e):
            run("sp", e)


class Builder:
    def __init__(self, T, depth, stages=("f0", "mx", "f1")):
        self.stages = stages
        self.T = T
        self.depth = depth
        self.ntiles = T // TT
        self.nc = bass.Bass("TRN2", target_bir_lowering=False)
        self.tr = Tracker()
        self.bank_i = 0
        self.held = set()
        self.cur_phase = "io"
        self.slab_i = 0
        self.uid = 0
        self.cast_i = 0
        self.tick_i = 0
        self.phase_i = 0
        self.cur_pieces = []
        self.deferred = []

    def dram_in(self, name, shape):
        return self.nc.dram_tensor(name, list(shape), F32, kind="ExternalInput").ap()

    def sb(self, name, shape, dt):
        return self.es.enter_context(self.nc.sbuf_tensor(name, list(shape), dt))

    def bank(self, hold=False):
        i = self.bank_i
        while i in self.held:
            i = (i + 1) % 8
        self.bank_i = (i + 1) % 8
        if hold:
            self.held.add(i)
        return ("ps", i), self.ps[i]

    def phase(self, name):
        if self.cur_phase != name:
            self.tr.arena_switch()
            self.cur_phase = name

    def PE(self, fn, R=(), W=()):
        self.tr.op("pe", fn, R, W)

    def ACT(self, fn, R=(), W=()):
        self.tr.op("act", fn, R, W)

    def DVE(self, fn, R=(), W=()):
        self.tr.op("dve", fn, R, W)

    def POOL(self, fn, R=(), W=()):
        self.tr.op("pool", fn, R, W)

    def mm(self, out, lhsT, rhs, start, stop, R, W):
        self.PE(lambda e: e.matmul(out, lhsT, rhs, start=start, stop=stop), R, W)

    def build(self):
        nc = self.nc
        depth = self.depth
        T = self.T
        with ExitStack() as es:
            self.es = es
            es.enter_context(nc.allow_non_contiguous_dma(reason="small param loads"))
            self.x = self.dram_in("x", [T, D])
            self.y = nc.dram_tensor("y", [T, D], F32, kind="ExternalOutput").ap()
            din = {}
            shapes = {
                "ffn1_norm": [depth, D], "ffn1_w_gate": [depth, D, F], "ffn1_w_up": [depth, D, F],
                "ffn1_w_down": [depth, F, D], "mix_norm": [depth, D], "w_in": [depth, D, PW],
                "w_a2": [depth, 16, 256], "b_a": [depth, 256], "gla_out_norm": [depth, 128],
                "conv_w": [depth, 3, 512], "w_out": [depth, D, D], "ffn2_norm": [depth, D],
                "ffn2_w_gate": [depth, D, F], "ffn2_w_up": [depth, D, F], "ffn2_w_down": [depth, F, D],
                "final_norm": [D],
            }
            for k, s in shapes.items():
                din[k] = self.dram_in(k, s)
            self.din = din
            big = ["ffn1_w_gate", "ffn1_w_up", "ffn1_w_down", "w_in", "w_out",
                   "ffn2_w_gate", "ffn2_w_up", "ffn2_w_down"]
            self.wbf = {k: nc.dram_tensor("bf_" + k, shapes[k], BF16, kind="Internal").ap() for k in big}

            self.xT = self.sb("xT", [P, DC, TT], F32)
            self.hT = self.sb("hT", [P, DC, TT], BF16)
            self.wb = self.sb("wb", [P, 12 * D], BF16)
            self.ring = self.sb("ring", [P, RING, 4096], BF16)
            self.wa = self.sb("wa", [P, DC, 16], BF16)
            self.rstd = self.sb("rstd", [P, TT], F32)
            self.sq = self.sb("sq", [P, 3, SUB], BF16)
            self.aaug = self.sb("aaug", [32, TT], BF16)
            self.dec = self.sb("dec", [P, 2, NCH], F32)
            self.S32 = self.sb("S32", [P, 2 * depth, 2, 128], F32)
            self.tails = self.sb("tails", [P, depth, 4, 2], F32)
            self.NG = 3 * depth * DC + DC + depth
            self.NCW = depth * 3 * 4
            assert self.NG <= P and self.NCW <= P
            self.prow = self.sb("prow", [P, P], F32)
            self.crow = self.sb("crow", [P, P], F32)
            self.gall = self.sb("gall", [P, self.NG], F32)
            self.cwall = self.sb("cwall", [P, self.NCW], F32)
            n8 = depth * DC
            self.gn1 = self.gall[:, 0:n8].rearrange("p (l c) -> p l c", l=depth)
            self.gnm = self.gall[:, n8:2 * n8].rearrange("p (l c) -> p l c", l=depth)
            self.gn2 = self.gall[:, 2 * n8:3 * n8].rearrange("p (l c) -> p l c", l=depth)
            self.gnf = self.gall[:, 3 * n8:3 * n8 + DC]
            self.gon = self.gall[:, 3 * n8 + DC:3 * n8 + DC + depth]
            self.cw = self.cwall[:, :].rearrange("p (l k c) -> p l k c", l=depth, k=3)
            self.wa2 = self.sb("wa2", [32, depth, 256], BF16)
            self.onesD = self.sb("onesD", [P, P], BF16)
            self.onesV = self.sb("onesV", [P, P], BF16)
            self.ident = self.sb("ident", [P, P], F32)
            self.triA = self.sb("triA", [P, P], F32)
            self.triB = self.sb("triB", [P, P], F32)
            self.mask4 = self.sb("mask4", [P, 4, P], F32)
            ARENA_BYTES = 86 * 1024
            self.arena = self.sb("arena", [P, ARENA_BYTES // 2], BF16)

            def carve(layout):
                off = 0
                for name, shape, dt, parts in layout:
                    n = 1
                    for d_ in shape:
                        n *= d_
                    nb = n * (4 if dt == F32 else 2)
                    v = self.arena[0:parts, off // 2:(off + nb) // 2]
                    if dt == F32:
                        v = v.bitcast(F32)
                    if len(shape) == 2:
                        v = v.rearrange("p (a b) -> p a b", a=shape[0])
                    elif len(shape) == 3:
                        v = v.rearrange("p (a b c) -> p a b c", a=shape[0], b=shape[1])
                    setattr(self, name, v)
                    off += (nb + 63) // 64 * 64
                assert off <= ARENA_BYTES, off
            carve([("act", (12, TT), BF16, P), ("tmpf", (3, SUB), F32, P)])
            carve([("stage", (2, D), F32, P), ("ostage", (2, D), F32, P), ("yT", (DC, P), F32, P),
                   ("wa2f", (depth, 256), F32, 32)])
            carve([("qt", (4, TT), BF16, P), ("kt", (2, TT), BF16, P), ("vtok", (NCH, 512), BF16, P),
                   ("khat", (NCH, 256), BF16, P), ("sg", (4, TT), BF16, P), ("mixT", (DC, TT), BF16, P),
                   ("Lb", (2, 512), F32, P), ("eb", (2, SUB), F32, P), ("enb", (2, SUB), F32, P),
                   ("ebd", (2, SUB), F32, P), ("AT", (2, 512), BF16, P), ("sqo", (2, 512), BF16, P),
                   ("rso", (2, 512), F32, P), ("cu", (2, 2 + SUB), F32, P), ("cacc", (2, SUB), F32, P),
                   ("Sbf", (NCH, 2, 128), BF16, P)])
            self.ps = [es.enter_context(nc.psum_tensor("ps%d" % i, [P, 512], F32)) for i in range(8)]

            self.prologue()
            self.param_transposes()
            for ti in range(self.ntiles):
                phases = []
                for l in range(depth):
                    phases += [("f0", l, (self.gn1, l, False)), ("mx", l, (self.gnm, l, False)), ("f1", l, (self.gn2, l, False))]
                phases.append(("fin", None, (self.gnf, None, True)))
                if ti == 0:
                    self.load_tile(ti)
                self.norm_s(phases[0][2], 0)
                self.defer(lambda spec=phases[0][2]: self.norm_s(spec, 1))
                for i, (kind, l, spec) in enumerate(phases[:-1]):
                    nxt = phases[i + 1][2]
                    if kind == "f0":
                        self.ffn(l, 0, ti, nxt)
                    elif kind == "mx":
                        self.mixer(l, ti, nxt)
                    else:
                        self.ffn(l, 1, ti, nxt)
                if ti + 1 < self.ntiles:
                    self.store_tile(ti, hook=lambda tc, ti=ti: self.load_tile(ti + 1, [tc - 4]) if tc >= 4 else None)
                    self.load_tile(ti + 1, range(4, NCH))
                else:
                    self.store_tile(ti)
            self.tr.final_wait("act")
            self.sbuf_left = nc.sbuf_bytes_remaining
            self.tr.emit(nc, es)
        return nc

    def prologue(self):
        depth = self.depth
        din = self.din
        self.pending = []
        for l in range(depth):
            for gname in ("f0", "mx", "f1"):
                pcs = self.cast_pieces(l, gname)
                if l == 0 and gname == "f0":
                    for pc in pcs:
                        self.issue_cast(pc, None)
                else:
                    self.pending.append(pcs)
        n8 = depth * DC

        def fnp(e, sem):
            e.dma_start(out=self.prow[0:n8, :], in_=din["ffn1_norm"].rearrange("l (c p) -> (l c) p", p=P)).then_inc(sem, 16)
            e.dma_start(out=self.prow[n8:2 * n8, :], in_=din["mix_norm"].rearrange("l (c p) -> (l c) p", p=P)).then_inc(sem, 16)
            e.dma_start(out=self.prow[2 * n8:3 * n8, :], in_=din["ffn2_norm"].rearrange("l (c p) -> (l c) p", p=P)).then_inc(sem, 16)
            e.dma_start(out=self.prow[3 * n8:3 * n8 + DC, :], in_=din["final_norm"].rearrange("(c p) -> c p", p=P)).then_inc(sem, 16)
            e.dma_start(out=self.prow[3 * n8 + DC:self.NG, :], in_=din["gla_out_norm"]).then_inc(sem, 16)
            e.dma_start(out=self.crow[0:self.NCW, :], in_=din["conv_w"].rearrange("l k (c p) -> (l k c) p", p=P)).then_inc(sem, 16)
        self.tr.dma("sp", "par", 6, fnp, W=["prow"])
        self._param_transpose_pending = True
        self.POOL(lambda e: e.memset(self.wa2f[:, :, :], 0.0), W=["wa2f"])

        def fna(e, sem):
            e.dma_start(out=self.wa2f[0:16, :, :], in_=din["w_a2"].rearrange("l r n -> r l n")).then_inc(sem, 16)
            e.dma_start(out=self.wa2f[16:17, :, :], in_=din["b_a"].rearrange("(o l) n -> o l n", o=1)).then_inc(sem, 16)
        self.tr.dma("sp", "par2", 2, fna, W=["wa2f"])
        self.POOL(lambda e: e.tensor_copy(out=self.wa2[:, :, :], in_=self.wa2f[:, :, :]), R=["wa2f"], W=["wa2"])
        self.POOL(lambda e: e.memset(self.onesD[:, :], 1.0 / D), W=["onesD"])
        self.POOL(lambda e: e.memset(self.onesV[:, :], 1.0 / 128.0), W=["onesV"])
        self.POOL(lambda e: e.memset(self.aaug[:, :], 1.0), W=[("aaug", 0), ("aaug", 1)])
        self.POOL(lambda e: e.memset(self.S32[:, :, :, :], 0.0),
                  W=[("S32", bf_, l, a_, b_) for bf_ in range(2) for l in range(depth) for a_ in range(2) for b_ in range(2)])
        self.POOL(lambda e: e.memset(self.tails[:, :, :, :], 0.0), W=[("tails", l) for l in range(depth)])
        self.POOL(lambda e: e.memset(self.ident[:, :], 1.0), W=["ident"])
        self.POOL(lambda e: e.affine_select(out=self.ident[:, :], in_=self.ident[:, :], pattern=[[-1, P]],
                                            compare_op=ALU.is_equal, fill=0.0, base=0, channel_multiplier=1),
                  R=["ident"], W=["ident"])
        self.POOL(lambda e: e.memset(self.triA[:, :], -1.0 / 16.0), W=["triA"])
        self.POOL(lambda e: e.affine_select(out=self.triA[:, :], in_=self.triA[:, :], pattern=[[1, P]],
                                            compare_op=ALU.is_ge, fill=0.0, base=0, channel_multiplier=-1),
                  R=["triA"], W=["triA"])
        self.POOL(lambda e: e.memset(self.triB[:, :], -1.0 / 16.0), W=["triB"])
        self.POOL(lambda e: e.affine_select(out=self.triB[:, :], in_=self.triB[:, :], pattern=[[-1, P]],
                                            compare_op=ALU.is_ge, fill=0.0, base=-1, channel_multiplier=1),
                  R=["triB"], W=["triB"])
        self.POOL(lambda e: e.memset(self.mask4[:, :, :], 1.0), W=["mask4"])
        self.POOL(lambda e: e.affine_select(out=self.mask4[:, :, :], in_=self.mask4[:, :, :], pattern=[[0, 4], [1, P]],
                                            compare_op=ALU.is_ge, fill=0.0, base=0, channel_multiplier=-1),
                  R=["mask4"], W=["mask4"])

    def cast_pieces(self, l, gname):
        din, wbf = self.din, self.wbf
        out = []
        if gname in ("f0", "f1"):
            pre = "ffn1" if gname == "f0" else "ffn2"
            for b in range(4):
                c0, c1 = b * 704, (b + 1) * 704
                out.append((("wbf", l, gname, "gu%d" % b),
                            [(wbf[pre + "_w_gate"][l][:, c0:c1], din[pre + "_w_gate"][l][:, c0:c1]),
                             (wbf[pre + "_w_up"][l][:, c0:c1], din[pre + "_w_up"][l][:, c0:c1])]))
                if b == 2:
                    out.append((("wbf", l, gname, "d0"),
                                [(wbf[pre + "_w_down"][l][0:1536, :], din[pre + "_w_down"][l][0:1536, :])]))
            out.append((("wbf", l, gname, "d1"),
                        [(wbf[pre + "_w_down"][l][1536:F, :], din[pre + "_w_down"][l][1536:F, :])]))
        else:
            for nm, c0, c1 in (("a", 1536, 1552), ("v", 512, 1024), ("qk", 0, 512), ("g", 1024, 1536),
                               ("u", 1552, 2064), ("b", 2064, 2576), ("c", 2576, 3088)):
                out.append((("wbf", l, gname, nm), [(wbf["w_in"][l][:, c0:c1], din["w_in"][l][:, c0:c1])]))
            out.append((("wbf", l, gname, "o"), [(wbf["w_out"][l], din["w_out"][l])]))
        return out

    def wkey(self, key):
        return key if (key[1], key[2]) == (0, "f0") else key[:3]

    def issue_cast(self, piece, tickkey):
        key, pairs = piece
        if (key[1], key[2]) == (0, "f0"):
            sem = "c0_%d" % self.cast_i
            self.cast_i += 1
        else:
            sem = "cg%d%s" % (key[1], key[2])
        key = self.wkey(key)

        def fn(e, s, pairs=pairs):
            for dst, src_ in pairs:
                e.dma_start(out=dst, in_=src_).then_inc(s, 16)
        self.tr.dma("pool", sem, len(pairs), fn, R=([tickkey] if tickkey is not None else []), W=[key])

    def begin_phase(self, nticks):
        self.cur_pieces = []
        if self.pending:
            if self.phase_i == 0:
                self.cur_pieces += self.pending.pop(0)
            if self.pending:
                self.cur_pieces += self.pending.pop(0)
        self.per_tick = -(-len(self.cur_pieces) // nticks) if self.cur_pieces else 0
        self.phase_i += 1

    def tick(self, flush=False):
        if not self.cur_pieces:
            return
        key = ("tick", self.tick_i)
        self.tick_i += 1
        self.tr.lastw[key] = ("pe", self.tr.nops["pe"] - 1)
        n = len(self.cur_pieces) if flush else self.per_tick
        for _ in range(min(n, len(self.cur_pieces))):
            self.issue_cast(self.cur_pieces.pop(0), key)

    def param_transposes(self):
        for rows, n, dst in ((self.prow, self.NG, self.gall), (self.crow, self.NCW, self.cwall)):
            bk, ps = self.bank()
            self.mm(ps[:, 0:n], rows[0:n, :], self.ident[0:n, 0:n], True, True, R=["prow", "ident"], W=[bk])
            self.ACT(lambda e, ps=ps, n=n, dst=dst: e.activation(out=dst[:, 0:n], in_=ps[:, 0:n], func=AF.Copy),
                     R=[bk], W=["par"])

    def load_tile(self, ti, tcs=None):
        tok0 = ti * TT
        for tc in (range(NCH) if tcs is None else tcs):
            slot = tc % 2
            src = self.x[tok0 + tc * P: tok0 + (tc + 1) * P, :]
            self.tr.dma("sp", "xin%d" % slot, 1,
                        lambda e, sem, slot=slot, src=src: e.dma_start(out=self.stage[:, slot, :], in_=src).then_inc(sem, 16),
                        W=[("stage", slot)])
            for g in range(2):
                bk, ps = self.bank()
                for j in range(4):
                    dc = 4 * g + j
                    self.PE(lambda e, ps=ps, j=j, dc=dc, slot=slot: e.transpose(
                        ps[:, j * P:(j + 1) * P], self.stage[:, slot, dc * P:(dc + 1) * P], self.ident[:, :]),
                        R=[("stage", slot), "ident"], W=[bk])
                self.ACT(lambda e, ps=ps, g=g, tc=tc: e.activation(
                    out=self.xT[:, 4 * g:4 * g + 4, tc * P:(tc + 1) * P],
                    in_=ps[:, :].rearrange("p (a b) -> p a b", a=4), func=AF.Copy),
                    R=[bk], W=[("xT", dc, tc // 4) for dc in range(4 * g, 4 * g + 4)])

    def store_tile(self, ti, hook=None):
        tok0 = ti * TT
        self.flush_deferred()
        self.phase("io")
        for tc in range(NCH):
            s = tc // 4
            slot = tc % 2
            for dc in range(DC):
                eng = self.DVE
                eng(lambda e, dc=dc, tc=tc: e.scalar_tensor_tensor(
                    out=self.yT[:, dc, :], in0=self.xT[:, dc, tc * P:(tc + 1) * P], scalar=self.gnf[:, dc:dc + 1],
                    in1=self.rstd[:, tc * P:(tc + 1) * P], op0=ALU.mult, op1=ALU.mult),
                    R=[("xT", dc, s), ("rstd", s), "par"], W=[("yT", dc)])
            for g in range(2):
                bk, ps = self.bank()
                for j in range(4):
                    dc = 4 * g + j
                    self.PE(lambda e, ps=ps, j=j, dc=dc: e.transpose(
                        ps[:, j * P:(j + 1) * P], self.yT[:, dc, :], self.ident[:, :]),
                        R=[("yT", dc), "ident"], W=[bk])
                self.ACT(lambda e, ps=ps, g=g, slot=slot: e.activation(
                    out=self.ostage[:, slot, g * 512:(g + 1) * 512], in_=ps[:, :], func=AF.Copy),
                    R=[bk], W=[("ostage", slot)])
            dst = self.y[tok0 + tc * P: tok0 + (tc + 1) * P, :]
            self.tr.dma("act", "out%d" % slot, 1,
                        lambda e, sem, slot=slot, dst=dst: e.dma_start(out=dst, in_=self.ostage[:, slot, :]).then_inc(sem, 16),
                        R=[("ostage", slot)])
            if hook is not None:
                hook(tc)

    def norm_s(self, spec, s):
        gain, l, final = spec
        cs = slice(s * SUB, (s + 1) * SUB)
        bk, ps = self.bank()
        for dc in range(DC):
            q = self.uid % 3
            self.uid += 1
            self.ACT(lambda e, dc=dc, q=q, cs=cs: e.activation(out=self.sq[:, q, :], in_=self.xT[:, dc, cs], func=AF.Square),
                     R=[("xT", dc, s)], W=[("sq", q)])
            self.mm(ps[:, :], self.onesD[:, :], self.sq[:, q, :], dc == 0, dc == DC - 1,
                    R=[("sq", q), "onesD"], W=[bk])
        self.ACT(lambda e, ps=ps, cs=cs: e.activation(out=self.rstd[:, cs], in_=ps[:, :], func=AF.Ln, bias=EPS),
                 R=[bk], W=[("rstd", s)])
        self.ACT(lambda e, cs=cs: e.activation(out=self.rstd[:, cs], in_=self.rstd[:, cs], func=AF.Exp, scale=-0.5),
                 R=[("rstd", s)], W=[("rstd", s)])
        if final:
            return
        for dc in range(DC):
            g_ap = gain[:, l, dc:dc + 1]
            self.DVE(lambda e, dc=dc, cs=cs, g_ap=g_ap: e.scalar_tensor_tensor(
                out=self.hT[:, dc, cs], in0=self.xT[:, dc, cs], scalar=g_ap, in1=self.rstd[:, cs],
                op0=ALU.mult, op1=ALU.mult),
                R=[("xT", dc, s), ("rstd", s), "par"], W=[("hT", dc, s)])

    def defer(self, fn):
        self.deferred.append(fn)

    def flush_deferred(self):
        d, self.deferred = self.deferred, []
        for fn in d:
            fn()

    def residual_tail(self, emit_group, nxt):
        for s in range(NS):
            for dc in range(DC):
                emit_group(s, dc)
                if s == 1 and dc == 1:
                    self.norm_s(nxt, 0)
        self.defer(lambda: self.norm_s(nxt, 1))

    def slab(self, parts, R):
        k = self.slab_i
        self.slab_i += 1
        slot = k % RING
        sl = self.ring[:, slot, :]

        def fn(e, sem, parts=parts, sl=sl):
            for dstf, src in parts:
                e.dma_start(out=dstf(sl), in_=src).then_inc(sem, 16)
        self.tr.dma("sp", "ring%d" % slot, len(parts), fn, R=R, W=[("ring", slot)])
        return ("ring", slot), sl

    def load_wb(self, view_fn, src, R):
        def fn(e, sem):
            e.dma_start(out=view_fn(self.wb[:, :]), in_=src).then_inc(sem, 16)
        self.tr.dma("act", "wb", 1, fn, R=R, W=["wb"])

    def ffn(self, l, which, ti, nxt):
        pre = "ffn1" if which == 0 else "ffn2"
        gname = "f0" if which == 0 else "f1"
        self.begin_phase(13)
        wg = self.wbf[pre + "_w_gate"][l]
        wu = self.wbf[pre + "_w_up"][l]
        wd = self.wbf[pre + "_w_down"][l]
        gain = self.gn1 if which == 0 else self.gn2
        self.phase("ffn")
        slab0 = 0
        for half, nsl in enumerate(HALF_SLABS):
            nfc = nsl * 2
            fc0 = slab0 * 2
            self.load_wb(lambda w, nfc=nfc: w[:, 0:nfc * D].rearrange("p (j d) -> p j d", j=nfc),
                         wd[fc0 * P:(fc0 + nfc) * P, :].rearrange("(j p) d -> p j d", p=P), R=[self.wkey(("wbf", l, gname, "d%d" % half))])
            def load_gu(si, slab0=slab0):
                col0 = (slab0 + si) * FS
                rk, sl = self.slab([
                    (lambda s_: s_[:, 0:2048].rearrange("p (k c) -> p k c", k=DC), wg[:, col0:col0 + FS].rearrange("(k p) c -> p k c", p=P)),
                    (lambda s_: s_[:, 2048:4096].rearrange("p (k c) -> p k c", k=DC), wu[:, col0:col0 + FS].rearrange("(k p) c -> p k c", p=P)),
                ], R=[self.wkey(("wbf", l, gname, "gu%d" % b)) for b in sorted({col0 // 704, (col0 + FS - 1) // 704})])
                gv = sl[:, 0:2048].rearrange("p (k c) -> p k c", k=DC)
                uv = sl[:, 2048:4096].rearrange("p (k c) -> p k c", k=DC)
                return rk, gv, uv

            def gu_item(slabinfo, si, c, s):
                rk, gv, uv = slabinfo
                fcl = si * 2 + c
                cs = slice(s * SUB, (s + 1) * SUB)
                bg, pg = self.bank()
                bu, pu = self.bank()
                for kc in range(DC):
                    self.mm(pg[:, :], gv[:, kc, c * P:(c + 1) * P], self.hT[:, kc, cs], kc == 0, kc == DC - 1,
                            R=[rk, ("hT", kc, s)], W=[bg])
                for kc in range(DC):
                    self.mm(pu[:, :], uv[:, kc, c * P:(c + 1) * P], self.hT[:, kc, cs], kc == 0, kc == DC - 1,
                            R=[rk, ("hT", kc, s)], W=[bu])
                q = self.uid % 3
                self.uid += 1
                self.ACT(lambda e, pg=pg, q=q: e.activation(out=self.tmpf[:, q, :], in_=pg[:, :], func=AF.Silu),
                         R=[bg], W=[("tmpf", q)])
                self.DVE(lambda e, pu=pu, q=q, fcl=fcl, cs=cs: e.tensor_tensor(
                    out=self.act[:, fcl, cs], in0=pu[:, :], in1=self.tmpf[:, q, :], op=ALU.mult),
                    R=[bu, ("tmpf", q)], W=[("act", fcl, s)])

            si0 = 0
            if half == 0:
                sA, sB = load_gu(0), load_gu(1)
                gu_item(sA, 0, 0, 0)
                gu_item(sA, 0, 1, 0)
                self.flush_deferred()
                gu_item(sB, 1, 0, 0)
                gu_item(sB, 1, 1, 0)
                self.tick()
                gu_item(sA, 0, 0, 1)
                gu_item(sA, 0, 1, 1)
                gu_item(sB, 1, 0, 1)
                gu_item(sB, 1, 1, 1)
                self.tick()
                si0 = 2
            for si in range(si0, nsl):
                info = load_gu(si)
                for c in range(2):
                    for s in range(NS):
                        gu_item(info, si, c, s)
                self.tick()
            wv = self.wb[:, 0:nfc * D].rearrange("p (j d) -> p j d", j=nfc)

            def down_group(s, dc, wv=wv, nfc=nfc):
                cs = slice(s * SUB, (s + 1) * SUB)
                bk, ps = self.bank()
                for j in range(nfc):
                    self.mm(ps[:, :], wv[:, j, dc * P:(dc + 1) * P], self.act[:, j, cs], j == 0, j == nfc - 1,
                            R=["wb", ("act", j, s)], W=[bk])
                self.DVE(lambda e, ps=ps, dc=dc, cs=cs: e.scalar_tensor_tensor(
                    out=self.xT[:, dc, cs], in0=ps[:, :], scalar=0.5, in1=self.xT[:, dc, cs],
                    op0=ALU.mult, op1=ALU.add),
                    R=[bk, ("xT", dc, s)], W=[("xT", dc, s)])
            if half == 0:
                for s in range(NS):
                    for dc in range(DC):
                        down_group(s, dc)
            else:
                self.residual_tail(down_group, nxt)
            self.tick(flush=(half == 1))
            slab0 += nsl

    def mixer(self, l, ti, nxt):
        def grp(*names):
            return [self.wkey(("wbf", l, "mx", nm)) for nm in names]
        self.begin_phase(16)
        win = self.wbf["w_in"][l]
        wout = self.wbf["w_out"][l]
        self.phase("mix")
        self.POOL(lambda e: e.memset(self.qt[:, :, :], 0.0), W=[("qt", kc2, s) for kc2 in range(2) for s in range(NS)])

        def wcols(c0, n):
            return win[:, c0:c0 + n].rearrange("(k p) c -> p k c", p=P)

        def fa(e, sem):
            e.dma_start(out=self.wa[:, :, :], in_=wcols(1536, 16)).then_inc(sem, 16)
        self.tr.dma("sp", "wa", 1, fa, R=grp("a"), W=["wa"])
        def alr(s):
            cs = slice(s * SUB, (s + 1) * SUB)
            bk, ps = self.bank()
            for kc in range(DC):
                self.mm(ps[0:16, :], self.wa[:, kc, :], self.hT[:, kc, cs], kc == 0, kc == DC - 1,
                        R=["wa", ("hT", kc, s)], W=[bk])
            self.ACT(lambda e, ps=ps, cs=cs: e.activation(out=self.aaug[0:16, cs], in_=ps[0:16, :], func=AF.Copy),
                     R=[bk], W=[("aaug", s)])
        alr(0)
        rv, slv = self.slab([(lambda s_: s_[:, :].rearrange("p (k c) -> p k c", k=DC), wcols(512, 512))], R=grp("v"))
        vv = slv[:, :].rearrange("p (k c) -> p k c", k=DC)
        rqk, slqk = self.slab([(lambda s_: s_[:, :].rearrange("p (k c) -> p k c", k=DC), wcols(0, 512))], R=grp("qk"))
        qkv = slqk[:, :].rearrange("p (k c) -> p k c", k=DC)

        def vproj(n):
            bk, ps = self.bank()
            for kc in range(DC):
                self.mm(ps[:, :], self.hT[:, kc, n * P:(n + 1) * P], vv[:, kc, :], kc == 0, kc == DC - 1,
                        R=[rv, ("hT", kc, n // 4)], W=[bk])
            self.ACT(lambda e, ps=ps, n=n: e.activation(out=self.vtok[:, n, :], in_=ps[:, :], func=AF.Copy),
                     R=[bk], W=[("vtok", n)])

        bt_banks = {}
        for pr in range(NCH // 2):
            s = pr // 2
            if pr == 1:
                self.flush_deferred()
            if pr == 2:
                alr(1)
            if pr % 2 == 0:
                for kc2 in range(2):
                    bt_banks[(s, kc2)] = self.bank(hold=True)
            bkz, psz = self.bank()
            for j in range(2):
                n = 2 * pr + j
                self.mm(psz[:, j * 256:(j + 1) * 256], self.aaug[:, n * P:(n + 1) * P], self.wa2[:, l, :], True, True,
                        R=[("aaug", s), "wa2"], W=[bkz])
            lq = pr % 2
            self.ACT(lambda e, psz=psz, lq=lq: e.activation(out=self.Lb[:, lq, :], in_=psz[:, :], func=AF.Exp, scale=-1.0),
                     R=[bkz], W=[("Lb", lq)])
            self.ACT(lambda e, lq=lq: e.activation(out=self.Lb[:, lq, :], in_=self.Lb[:, lq, :], func=AF.Ln, bias=1.0),
                     R=[("Lb", lq)], W=[("Lb", lq)])
            vproj(2 * pr)
            vproj(2 * pr + 1)
            bkd, psd = self.bank()
            for j in range(2):
                n = 2 * pr + j
                nl = n % 4
                Lv = self.Lb[:, lq, j * 256:(j + 1) * 256]
                for kc2 in range(2):
                    bkt, pst = bt_banks[(s, kc2)]
                    self.mm(pst[:, nl * P:(nl + 1) * P], Lv[:, kc2 * P:(kc2 + 1) * P], self.triA[:, :], True, True,
                            R=[("Lb", lq), "triA"], W=[bkt])
                self.mm(psd[:, j * 256:(j + 1) * 256], self.triB[:, :], Lv, True, True,
                        R=[("Lb", lq), "triB"], W=[bkd])
            bkk, psk = self.bank()
            for j in range(2):
                n = 2 * pr + j
                for kc in range(DC):
                    self.mm(psk[:, j * 256:(j + 1) * 256], self.hT[:, kc, n * P:(n + 1) * P], qkv[:, kc, 256:512],
                            kc == 0, kc == DC - 1, R=[rqk, ("hT", kc, s)], W=[bkk])
            eq = pr % 2
            self.ACT(lambda e, psd=psd, eq=eq: e.activation(out=self.ebd[:, eq, :], in_=psd[:, :], func=AF.Exp),
                     R=[bkd], W=[("ebd", eq)])
            self.DVE(lambda e, psk=psk, eq=eq, pr=pr: e.tensor_tensor(
                out=self.khat[:, 2 * pr:2 * pr + 2, :], in0=psk[:, :].rearrange("p (a b) -> p a b", a=2),
                in1=self.ebd[:, eq, :].rearrange("p (a b) -> p a b", a=2), op=ALU.mult),
                R=[bkk, ("ebd", eq)], W=[("khat", 2 * pr), ("khat", 2 * pr + 1)])
            if pr % 2 == 1:
                cs = slice(s * SUB, (s + 1) * SUB)
                for kc2 in range(2):
                    bkt, pst = bt_banks[(s, kc2)]
                    self.ACT(lambda e, pst=pst, kc2=kc2: e.activation(out=self.eb[:, kc2, :], in_=pst[:, :], func=AF.Exp),
                             R=[bkt], W=[("eb", kc2)])
                    self.ACT(lambda e, pst=pst, kc2=kc2: e.activation(out=self.enb[:, kc2, :], in_=pst[:, :], func=AF.Exp, scale=-1.0),
                             R=[bkt], W=[("enb", kc2)])
                    self.DVE(lambda e, kc2=kc2, s=s: e.tensor_copy(out=self.dec[:, kc2, 4 * s:4 * s + 4], in_=self.eb[:, kc2, 127:512:128]),
                             R=[("eb", kc2)], W=[("dec", kc2, s)])
                    bq, pq = self.bank()
                    for kc in range(DC):
                        self.mm(pq[:, :], qkv[:, kc, kc2 * P:(kc2 + 1) * P], self.hT[:, kc, cs], kc == 0, kc == DC - 1,
                                R=[rqk, ("hT", kc, s)], W=[bq])
                    for hl in range(2):
                        pr_ = slice(hl * 64, (hl + 1) * 64)
                        self.DVE(lambda e, pq=pq, kc2=kc2, cs=cs, pr_=pr_, hl=hl: e.scalar_tensor_tensor(
                            out=self.qt[pr_, 2 * kc2 + hl, cs], in0=pq[pr_, :], scalar=0.125, in1=self.eb[pr_, kc2, :],
                            op0=ALU.mult, op1=ALU.mult),
                            R=[bq, ("eb", kc2)], W=[("qt", kc2, s)])
                    bk_, pk = self.bank()
                    for kc in range(DC):
                        self.mm(pk[:, :], qkv[:, kc, 256 + kc2 * P:256 + (kc2 + 1) * P], self.hT[:, kc, cs], kc == 0, kc == DC - 1,
                                R=[rqk, ("hT", kc, s)], W=[bk_])
                    self.DVE(lambda e, pk=pk, kc2=kc2, cs=cs: e.tensor_tensor(
                        out=self.kt[:, kc2, cs], in0=pk[:, :], in1=self.enb[:, kc2, :], op=ALU.mult),
                        R=[bk_, ("enb", kc2)], W=[("kt", kc2, s)])
                    self.held.discard(bkt[1])
            self.tick()
        rg, slg = self.slab([(lambda s_: s_[:, :].rearrange("p (k c) -> p k c", k=DC), wcols(1024, 512))], R=grp("g"))
        gvw = slg[:, :].rearrange("p (k c) -> p k c", k=DC)
        for n in range(NCH):
            s = n // 4
            bku, psu = self.bank()
            for kc2 in range(2):
                self.mm(psu[:, kc2 * 256:(kc2 + 1) * 256], self.khat[:, n, kc2 * P:(kc2 + 1) * P],
                        self.vtok[:, n, kc2 * 256:(kc2 + 1) * 256], True, True,
                        R=[("khat", n), ("vtok", n)], W=[bku])
            cur, nxb = n % 2, (n + 1) % 2
            ic, io = cur * self.depth + l, nxb * self.depth + l
            self.POOL(lambda e, n=n, ic=ic: e.tensor_copy(out=self.Sbf[:, n, :, :], in_=self.S32[:, ic, :, :]),
                      R=[("S32", cur, l, a_, b_) for a_ in range(2) for b_ in range(2)], W=[("Sbf", n)])
            for kc2 in range(2):
                for hl in range(2):
                    pr_ = slice(hl * 64, (hl + 1) * 64)
                    c0 = kc2 * 256 + hl * 128
                    self.DVE(lambda e, psu=psu, kc2=kc2, pr_=pr_, c0=c0, n=n, ic=ic, io=io: e.scalar_tensor_tensor(
                        out=self.S32[pr_, io, kc2, :], in0=self.S32[pr_, ic, kc2, :], scalar=self.dec[pr_, kc2, n:n + 1],
                        in1=psu[pr_, c0:c0 + 128], op0=ALU.mult, op1=ALU.add),
                        R=[bku, ("S32", cur, l, kc2, hl), ("dec", kc2, s)], W=[("S32", nxb, l, kc2, hl)])
            sg_s, sg_c = n // 4, n % 4
            cs = slice(sg_s * SUB, (sg_s + 1) * SUB)
            bk, ps = self.bank()
            for kc in range(DC):
                self.mm(ps[:, :], gvw[:, kc, sg_c * P:(sg_c + 1) * P], self.hT[:, kc, cs], kc == 0, kc == DC - 1,
                        R=[rg, ("hT", kc, sg_s)], W=[bk])
            self.ACT(lambda e, ps=ps, sg_c=sg_c, cs=cs: e.activation(out=self.sg[:, sg_c, cs], in_=ps[:, :], func=AF.Silu),
                     R=[bk], W=[("sg", sg_c, sg_s)])
        self.tick()
        self.load_wb(lambda w: w[:, 0:DC * D].rearrange("p (k d) -> p k d", k=DC),
                     wout.rearrange("(k p) d -> p k d", p=P), R=grp("o"))
        wov = self.wb[:, 0:DC * D].rearrange("p (k d) -> p k d", k=DC)

        def conv_item(c, s):
            rc, slc = self.slab([
                (lambda s_, j=j: s_[:, j * 1024:(j + 1) * 1024].rearrange("p (k c) -> p k c", k=DC),
                 wcols(1552 + 512 * j + c * P, P)) for j in range(3)], R=grp("u", "b", "c"))
            cs = slice(s * SUB, (s + 1) * SUB)
            pss = []
            for j in range(3):
                wvj = slc[:, j * 1024:(j + 1) * 1024].rearrange("p (k c) -> p k c", k=DC)
                bk, ps = self.bank()
                for kc in range(DC):
                    self.mm(ps[:, :], wvj[:, kc, :], self.hT[:, kc, cs], kc == 0, kc == DC - 1,
                            R=[rc, ("hT", kc, s)], W=[bk])
                pss.append((bk, ps))
            (bu, pu), (bb, pb), (bc, pc) = pss
            q = self.uid % 2
            self.uid += 1
            self.ACT(lambda e, pc=pc, q=q: e.activation(out=self.cacc[:, q, :], in_=pc[:, :], func=AF.Copy),
                     R=[bc], W=[("cacc", q)])
            self.ACT(lambda e, pb=pb, q=q: e.activation(out=self.ebd[:, q, :], in_=pb[:, :], func=AF.Copy),
                     R=[bb], W=[("ebd", q)])
            self.POOL(lambda e, q=q, c=c: e.tensor_copy(out=self.cu[:, q, 0:2], in_=self.tails[:, l, c, :]),
                      R=[("tails", l, c)], W=[("cu", q)])
            self.DVE(lambda e, pu=pu, q=q: e.tensor_tensor(out=self.cu[:, q, 2:2 + SUB], in0=pu[:, :], in1=self.cacc[:, q, :], op=ALU.mult),
                     R=[bu, ("cacc", q), ("cu", q)], W=[("cu", q)])
            self.POOL(lambda e, q=q, c=c: e.tensor_copy(out=self.tails[:, l, c, :], in_=self.cu[:, q, SUB:SUB + 2]),
                      R=[("cu", q)], W=[("tails", l, c)])
            self.ACT(lambda e, q=q, c=c: e.activation(out=self.cacc[:, q, :], in_=self.cu[:, q, 2:2 + SUB], func=AF.Copy,
                                                      scale=self.cw[:, l, 2, c:c + 1]),
                     R=[("cu", q), "par"], W=[("cacc", q)])
            self.DVE(lambda e, q=q, c=c: e.scalar_tensor_tensor(out=self.cacc[:, q, :], in0=self.cu[:, q, 1:1 + SUB],
                                                                scalar=self.cw[:, l, 1, c:c + 1], in1=self.cacc[:, q, :],
                                                                op0=ALU.mult, op1=ALU.add),
                     R=[("cu", q), ("cacc", q), "par"], W=[("cacc", q)])
            self.DVE(lambda e, q=q, c=c: e.scalar_tensor_tensor(out=self.cacc[:, q, :], in0=self.cu[:, q, 0:SUB],
                                                                scalar=self.cw[:, l, 0, c:c + 1], in1=self.cacc[:, q, :],
                                                                op0=ALU.mult, op1=ALU.add),
                     R=[("cu", q), ("cacc", q), "par"], W=[("cacc", q)])
            self.DVE(lambda e, q=q, c=c, cs=cs: e.tensor_tensor(out=self.mixT[:, 4 + c, cs], in0=self.ebd[:, q, :], in1=self.cacc[:, q, :], op=ALU.mult),
                     R=[("ebd", q), ("cacc", q)], W=[("mixT", 4 + c, s)])

        def gla_scores(n):
            s = n // 4
            cn = slice(n * P, (n + 1) * P)
            bsc, psc = self.bank()
            for h in range(4):
                kc2 = h // 2
                self.mm(psc[:, h * P:(h + 1) * P], self.kt[:, kc2, cn], self.qt[:, h, cn], True, True,
                        R=[("kt", kc2, s), ("qt", kc2, s)], W=[bsc])
            q = n % 2
            self.DVE(lambda e, psc=psc, q=q: e.tensor_tensor(
                out=self.AT[:, q, :], in0=psc[:, :], in1=self.mask4[:, :, :].rearrange("p a b -> p (a b)"), op=ALU.mult),
                R=[bsc, "mask4"], W=[("AT", q)])

        def gla_out(n):
            s = n // 4
            cn = slice(n * P, (n + 1) * P)
            q = n % 2
            bo, po = self.bank()
            for h in range(4):
                kc2 = h // 2
                self.mm(po[:, h * P:(h + 1) * P], self.vtok[:, n, h * P:(h + 1) * P], self.AT[:, q, h * P:(h + 1) * P], True, False,
                        R=[("vtok", n), ("AT", q)], W=[bo])
                self.mm(po[:, h * P:(h + 1) * P], self.Sbf[:, n, kc2, :], self.qt[:, h, cn], False, True,
                        R=[("Sbf", n), ("qt", kc2, s)], W=[bo])
            self.ACT(lambda e, po=po, q=q: e.activation(out=self.sqo[:, q, :], in_=po[:, :], func=AF.Square),
                     R=[bo], W=[("sqo", q)])
            self.ACT(lambda e, po=po, q=q: e.activation(out=self.Lb[:, q, :], in_=po[:, :], func=AF.Copy),
                     R=[bo], W=[("Lb", q)])

        def gla_norm(n):
            s = n // 4
            cn = slice(n * P, (n + 1) * P)
            q = n % 2
            bst, pst = self.bank()
            self.mm(pst[:, :], self.onesV[:, :], self.sqo[:, q, :], True, True, R=[("sqo", q), "onesV"], W=[bst])
            self.ACT(lambda e, pst=pst, q=q: e.activation(out=self.rso[:, q, :], in_=pst[:, :], func=AF.Ln, bias=EPS),
                     R=[bst], W=[("rso", q)])
            self.ACT(lambda e, q=q: e.activation(out=self.rso[:, q, :], in_=self.rso[:, q, :], func=AF.Exp, scale=-0.5),
                     R=[("rso", q)], W=[("rso", q)])
            self.DVE(lambda e, q=q: e.scalar_tensor_tensor(out=self.rso[:, q, :], in0=self.Lb[:, q, :], scalar=self.gon[:, l:l + 1],
                                                           in1=self.rso[:, q, :], op0=ALU.mult, op1=ALU.mult),
                     R=[("Lb", q), ("rso", q), "par"], W=[("rso", q)])
            self.POOL(lambda e, q=q, cn=cn: e.tensor_tensor(
                out=self.mixT[:, 0:4, cn], in0=self.rso[:, q, :].rearrange("p (a b) -> p a b", a=4), in1=self.sg[:, :, cn], op=ALU.mult),
                R=[("rso", q)] + [("sg", c, s) for c in range(4)], W=[("mixT", c, s) for c in range(4)])

        def wout_group(s, dc):
            cs = slice(s * SUB, (s + 1) * SUB)
            bk, ps = self.bank()
            for kc in range(DC):
                self.mm(ps[:, :], wov[:, kc, dc * P:(dc + 1) * P], self.mixT[:, kc, cs], kc == 0, kc == DC - 1,
                        R=["wb", ("mixT", kc, s)], W=[bk])
            self.DVE(lambda e, ps=ps, dc=dc, cs=cs: e.tensor_tensor(out=self.xT[:, dc, cs], in0=ps[:, :], in1=self.xT[:, dc, cs], op=ALU.add),
                     R=[bk, ("xT", dc, s)], W=[("xT", dc, s)])

        gla_scores(0)
        for i in range(NCH + 1):
            if i + 1 < NCH:
                gla_scores(i + 1)
            if i < NCH:
                gla_out(i)
            if i >= 1:
                conv_item((i - 1) % 4, (i - 1) // 4)
                gla_norm(i - 1)
            for dc in {5: (0,), 6: (1,), 7: (2, 3), 8: (4, 5, 6, 7)}.get(i, ()):
                wout_group(0, dc)
            self.tick()
        for dc in range(DC):
            wout_group(1, dc)
            if dc == 1:
                self.norm_s(nxt, 0)
        self.defer(lambda: self.norm_s(nxt, 1))
        self.tick(flush=True)


_W_NAMES = ["ffn1_norm", "ffn1_w_gate", "ffn1_w_up", "ffn1_w_down", "mix_norm", "w_in", "w_a2", "b_a",
            "gla_out_norm", "conv_w", "w_out", "ffn2_norm", "ffn2_w_gate", "ffn2_w_up", "ffn2_w_down", "final_norm"]


def run(inputs, trace=False, stages=("f0", "mx", "f1")):
    x = np.ascontiguousarray(inputs["x"], dtype=np.float32)
    B, T, _ = x.shape
    depth = inputs["w_in"].shape[0]
    nc = Builder(T, depth, stages).build()
    w = {k: np.ascontiguousarray(inputs[k], dtype=np.float32) for k in _W_NAMES}
    in_maps = [dict(w, x=x[b]) for b in range(B)]
    res = run_bass_kernel_spmd(nc, in_maps, core_ids=list(range(B)), trace=trace)
    out = np.stack([res.results[b]["y"] for b in range(B)], axis=0)
    return out, res


def kernel(**inputs):
    out, _ = run(inputs)
    return out
```
